# Optimizing a Trainium2 kernel written in Bass

```python
import jax, jax.numpy as jnp
from jax import lax
import numpy as np

D_MODEL = 1024
BATCH = 4
SEQ = 8192
DEPTH = 1

ATTN_HEADS = 8
ATTN_HD = 64
ATTN_WIDTH = ATTN_HEADS * ATTN_HD
DILATED_PATTERNS = ((128, 1), (512, 4), (2048, 16))
ATTN_BLOCK = 128
DN_HEADS = 4
DN_KD = 128
DN_VD = 128
DN_QK_WIDTH = DN_HEADS * DN_KD
DN_V_WIDTH = DN_HEADS * DN_VD
DN_CONV_DIM = 2 * DN_QK_WIDTH + DN_V_WIDTH
CONV_K = 4
DN_CHUNK = 64
D_MIX = ATTN_WIDTH + DN_V_WIDTH
IN_SPLITS = (ATTN_WIDTH, 2 * ATTN_WIDTH, 3 * ATTN_WIDTH,
             3 * ATTN_WIDTH + DN_CONV_DIM,
             3 * ATTN_WIDTH + DN_CONV_DIM + DN_V_WIDTH,
             3 * ATTN_WIDTH + DN_CONV_DIM + DN_V_WIDTH + DN_HEADS)
IN_COLS = 3 * ATTN_WIDTH + DN_CONV_DIM + DN_V_WIDTH + 2 * DN_HEADS
N_EXPERTS = 32
TOP_K = 4
D_EXPERT = 1024
SWIGLU_LIMIT = 7.0
SWIGLU_ALPHA = 1.702
MOE_BLOCK = 128
NORM_EPS = 1e-6

kernel_name = 'hybrid_dilated_attn_gdn_moe_adaln'


def rms_norm(x, w):
    xf = x.astype(jnp.float32)
    y = xf * lax.rsqrt(jnp.mean(xf * xf, axis=-1, keepdims=True) + NORM_EPS)
    return (y * w.astype(jnp.float32)).astype(x.dtype)


def l2_norm(x):
    return x * lax.rsqrt(jnp.sum(x * x, axis=-1, keepdims=True) + NORM_EPS)


def modulate(x, w, shift, scale):
    return rms_norm(x, w) * (1 + scale[:, None, :]) + shift[:, None, :]


def dilated_branch(q, k, v, window, dilation):
    B, S, H, Dh = q.shape
    L = S // dilation
    span = window // dilation
    nb = -(-L // ATTN_BLOCK)
    Lp = nb * ATTN_BLOCK

    def to_sub(t):
        t = t.astype(jnp.float32).reshape(B, L, dilation, H, Dh).transpose(0, 2, 3, 1, 4)
        return jnp.pad(t, ((0, 0), (0, 0), (0, 0), (0, Lp - L), (0, 0)))

    def window_blocks(t):
        t = jnp.pad(t, ((0, 0), (0, 0), (0, 0), (ATTN_BLOCK, 0), (0, 0)))
        t = t.reshape(B, dilation, H, nb + 1, ATTN_BLOCK, Dh)
        return jnp.concatenate([t[:, :, :, :-1], t[:, :, :, 1:]], axis=-2)

    qb = to_sub(q).reshape(B, dilation, H, nb, ATTN_BLOCK, Dh)
    kw = window_blocks(to_sub(k))
    vw = window_blocks(to_sub(v))
    s = jnp.einsum('bdhnqe,bdhnke->bdhnqk', qb, kw) * (ATTN_HD ** -0.5)
    dist = (jnp.arange(ATTN_BLOCK)[:, None] + ATTN_BLOCK) - jnp.arange(2 * ATTN_BLOCK)[None, :]
    band = (dist >= 0) & (dist <= span)
    kpos = jnp.arange(nb)[:, None] * ATTN_BLOCK - ATTN_BLOCK + jnp.arange(2 * ATTN_BLOCK)[None, :]
    mask = band[None, :, :] & (kpos >= 0)[:, None, :]
    s = jnp.where(mask, s, -jnp.inf)
    m = jnp.max(s, axis=-1, keepdims=True)
    p = jnp.exp(s - m)
    den = jnp.sum(p, axis=-1)
    o = jnp.einsum('bdhnqk,bdhnke->bdhnqe', p, vw) / den[..., None]
    lse = m[..., 0] + jnp.log(den)
    o = o.reshape(B, dilation, H, Lp, Dh)[:, :, :, :L].transpose(0, 3, 1, 2, 4).reshape(B, S, H, Dh)
    lse = lse.reshape(B, dilation, H, Lp)[:, :, :, :L].transpose(0, 3, 1, 2).reshape(B, S, H)
    return o, lse


def dilated_attention(q, k, v):
    outs, lses = [], []
    for window, dilation in DILATED_PATTERNS:
        o, lse = dilated_branch(q, k, v, window, dilation)
        outs.append(o)
        lses.append(lse)
    wts = jax.nn.softmax(jnp.stack(lses, axis=0), axis=0)
    return jnp.einsum('gbsh,gbshe->bshe', wts, jnp.stack(outs, axis=0))


def causal_short_conv(x, w):
    S = x.shape[1]
    xp = jnp.pad(x, ((0, 0), (CONV_K - 1, 0), (0, 0)))
    y = w[CONV_K - 1] * x
    for j in range(CONV_K - 1):
        y = y + w[j] * xp[:, j:j + S]
    return y


def gated_delta_rule(q, k, v, g, beta):
    B, S, H, Dk = q.shape
    Dv = v.shape[-1]
    C = DN_CHUNK
    N = S // C
    q = l2_norm(q) * (Dk ** -0.5)
    k = l2_norm(k)

    def chunk(t):
        return jnp.swapaxes(t.reshape((B, N, C, H) + t.shape[3:]), 2, 3)

    qc, kc, vc, gc, bc = chunk(q), chunk(k), chunk(v), chunk(g), chunk(beta)
    gc = jnp.cumsum(gc, axis=-1)
    tril = jnp.tril(jnp.ones((C, C), dtype=bool))
    strict = jnp.tril(jnp.ones((C, C), dtype=bool), -1)
    decay = jnp.exp(jnp.where(tril, gc[..., :, None] - gc[..., None, :], -jnp.inf))
    kb = kc * bc[..., None]
    vb = vc * bc[..., None]
    a_low = jnp.where(strict, jnp.einsum('bnhie,bnhje->bnhij', kb, kc) * decay, 0.0)
    lhs = a_low + jnp.eye(C, dtype=jnp.float32)
    u = lax.linalg.triangular_solve(lhs, vb, left_side=True, lower=True)
    w = lax.linalg.triangular_solve(lhs, kb * jnp.exp(gc)[..., None], left_side=True, lower=True)
    intra = jnp.einsum('bnhie,bnhje->bnhij', qc, kc) * decay
    q_dec = qc * jnp.exp(gc)[..., None]
    k_dec = kc * jnp.exp(gc[..., -1:] - gc)[..., None]
    g_last = jnp.exp(gc[..., -1])

    def step(state, xs):
        qd, kd, u_i, w_i, intra_i, gl = xs
        v_new = u_i - jnp.einsum('bhce,bhef->bhcf', w_i, state)
        o = jnp.einsum('bhce,bhef->bhcf', qd, state) + jnp.einsum('bhij,bhjf->bhif', intra_i, v_new)
        state = state * gl[..., None, None] + jnp.einsum('bhce,bhcf->bhef', kd, v_new)
        return state, o

    xs = tuple(jnp.moveaxis(t, 1, 0) for t in (q_dec, k_dec, u, w, intra, g_last))
    state0 = jnp.zeros((B, H, Dk, Dv), jnp.float32)
    _, o = lax.scan(step, state0, xs)
    return o.transpose(1, 0, 3, 2, 4).reshape(B, S, H, Dv)


def hybrid_mixer(h, w_in, conv_w, A_log, dt_bias, dn_norm_w, w_out):
    B, S, _ = h.shape
    proj = h @ w_in
    aq, ak, av, dqkv, dz, db, da = jnp.split(proj, IN_SPLITS, axis=-1)
    shp_a = (B, S, ATTN_HEADS, ATTN_HD)
    attn_out = dilated_attention(aq.reshape(shp_a), ak.reshape(shp_a), av.reshape(shp_a))
    attn_out = attn_out.reshape(B, S, ATTN_WIDTH).astype(h.dtype)
    dqkv = jax.nn.silu(causal_short_conv(dqkv, conv_w)).astype(jnp.float32)
    dq, dk, dv = jnp.split(dqkv, (DN_QK_WIDTH, 2 * DN_QK_WIDTH), axis=-1)
    g = -jnp.exp(A_log.astype(jnp.float32)) * jax.nn.softplus(da.astype(jnp.float32) + dt_bias.astype(jnp.float32))
    beta = jax.nn.sigmoid(db.astype(jnp.float32))
    o = gated_delta_rule(dq.reshape(B, S, DN_HEADS, DN_KD), dk.reshape(B, S, DN_HEADS, DN_KD),
                         dv.reshape(B, S, DN_HEADS, DN_VD), g, beta)
    o = rms_norm(o, dn_norm_w) * jax.nn.silu(dz.astype(jnp.float32).reshape(B, S, DN_HEADS, DN_VD))
    dn_out = o.reshape(B, S, DN_V_WIDTH).astype(h.dtype)
    return jnp.concatenate([attn_out, dn_out], axis=-1) @ w_out


def clamped_swiglu(hgu):
    gate, up = jnp.split(hgu, 2, axis=-1)
    gate = jnp.minimum(gate, SWIGLU_LIMIT)
    up = jnp.clip(up, -SWIGLU_LIMIT, SWIGLU_LIMIT)
    return gate * jax.nn.sigmoid(SWIGLU_ALPHA * gate) * (up + 1)


def routed_moe(h, w_router, b_router, w1, b1, w2, b2):
    B, S, D = h.shape
    T = B * S
    xt = h.reshape(T, D)
    logits = (xt @ w_router + b_router).astype(jnp.float32)
    top_v, top_e = lax.top_k(logits, TOP_K)
    gates = jax.nn.softmax(top_v, axis=-1)
    A = T * TOP_K
    e_flat = top_e.reshape(A)
    tok_flat = jnp.repeat(jnp.arange(T, dtype=jnp.int32), TOP_K)
    g_flat = gates.reshape(A)
    order = jnp.argsort(e_flat)
    se, stok, sg = e_flat[order], tok_flat[order], g_flat[order]
    counts = jnp.zeros((N_EXPERTS,), jnp.int32).at[e_flat].add(1)
    padded = (counts + MOE_BLOCK - 1) // MOE_BLOCK * MOE_BLOCK
    pend = jnp.cumsum(padded)
    pstart = pend - padded
    cstart = jnp.cumsum(counts) - counts
    dest = pstart[se] + (jnp.arange(A, dtype=jnp.int32) - cstart[se])
    P = A + N_EXPERTS * MOE_BLOCK
    NB = P // MOE_BLOCK
    row_tok = jnp.zeros((P,), jnp.int32).at[dest].set(stok)
    row_gate = jnp.zeros((P,), jnp.float32).at[dest].set(sg)
    block_exp = jnp.minimum(jnp.searchsorted(pend, jnp.arange(NB) * MOE_BLOCK, side='right'),
                            N_EXPERTS - 1).astype(jnp.int32)
    xs = xt[row_tok].reshape(NB, MOE_BLOCK, D)

    def expert_block(args):
        xb, e = args
        return clamped_swiglu(xb @ w1[e] + b1[e]) @ w2[e] + b2[e]

    yb = lax.map(expert_block, (xs, block_exp)).reshape(P, D)
    y = jnp.zeros((T, D), h.dtype).at[row_tok].add(yb * row_gate[:, None].astype(yb.dtype))
    return y.reshape(B, S, D)


def setup_inputs(seed: int = 0) -> dict:
    key = jax.random.key(seed)
    ks = jax.random.split(key, 20)
    D, L, E, F = D_MODEL, DEPTH, N_EXPERTS, D_EXPERT
    nrm = jax.random.normal
    return {
        'x': nrm(ks[0], (BATCH, SEQ, D), jnp.float32),
        'c': nrm(ks[1], (BATCH, D), jnp.float32),
        'w_ada': nrm(ks[2], (L, D, 6 * D), jnp.float32) * (0.5 * D ** -0.5),
        'b_ada': nrm(ks[3], (L, 6 * D), jnp.float32) * 0.02,
        'norm1_w': 1.0 + 0.05 * nrm(ks[4], (L, D), jnp.float32),
        'w_in': nrm(ks[5], (L, D, IN_COLS), jnp.float32) * (D ** -0.5),
        'conv_w': nrm(ks[6], (L, CONV_K, DN_CONV_DIM), jnp.float32) * 0.5,
        'A_log': jnp.log(jax.random.uniform(ks[7], (L, DN_HEADS), jnp.float32, 1.0, 16.0)),
        'dt_bias': jax.random.uniform(ks[8], (L, DN_HEADS), jnp.float32, -4.5, -2.5),
        'dn_norm_w': 1.0 + 0.05 * nrm(ks[9], (L, DN_VD), jnp.float32),
        'w_out': nrm(ks[10], (L, D_MIX, D), jnp.float32) * (D_MIX ** -0.5),
        'norm2_w': 1.0 + 0.05 * nrm(ks[11], (L, D), jnp.float32),
        'w_router': nrm(ks[12], (L, D, E), jnp.float32) * (D ** -0.5),
        'b_router': nrm(ks[13], (L, E), jnp.float32) * 0.01,
        'w1': nrm(ks[14], (L, E, D, 2 * F), jnp.float32) * (D ** -0.5),
        'b1': nrm(ks[15], (L, E, 2 * F), jnp.float32) * 0.01,
        'w2': nrm(ks[16], (L, E, F, D), jnp.float32) * (F ** -0.5),
        'b2': nrm(ks[17], (L, E, D), jnp.float32) * 0.01,
        'final_norm_w': 1.0 + 0.05 * nrm(ks[18], (D,), jnp.float32),
    }


def reference(x, c, w_ada, b_ada, norm1_w, w_in, conv_w, A_log, dt_bias, dn_norm_w, w_out,
              norm2_w, w_router, b_router, w1, b1, w2, b2, final_norm_w):
    cond = jax.nn.silu(c)
    for l in range(DEPTH):
        mod = cond @ w_ada[l] + b_ada[l]
        shift1, scale1, gate1, shift2, scale2, gate2 = jnp.split(mod, 6, axis=-1)
        h = modulate(x, norm1_w[l], shift1, scale1)
        x = x + gate1[:, None, :] * hybrid_mixer(h, w_in[l], conv_w[l], A_log[l], dt_bias[l],
                                                 dn_norm_w[l], w_out[l])
        h = modulate(x, norm2_w[l], shift2, scale2)
        x = x + gate2[:, None, :] * routed_moe(h, w_router[l], b_router[l], w1[l], b1[l], w2[l], b2[l])
    return rms_norm(x, final_norm_w)
```

```python
import os
import numpy as np
import concourse.bass as bass
import concourse.mybir as mybir
from concourse.bass_utils import run_bass_kernel_spmd

F32 = mybir.dt.float32
BF16 = mybir.dt.bfloat16
AF = mybir.ActivationFunctionType
ALU = mybir.AluOpType
AX = mybir.AxisListType

NPRE = 4096
NMAIN = 4096
NEXT = NPRE + NMAIN
EPS = 1e-6
NEG = -30000.0
SEM_LIMIT = int(os.environ.get("K_SEMLIM", "8000"))


class Prog:
    def __init__(self, nc):
        self.nc = nc
        self.engs = ["pe", "act", "dve", "pool", "sp"]
        self.ops = {e: [] for e in self.engs}
        self.cnt = {e: 0 for e in self.engs}
        self.epoch = {e: 0 for e in self.engs}
        self.esem = {e: nc.alloc_semaphore(f"s_{e}_0") for e in self.engs}
        self.waited = {e: {} for e in self.engs}
        self.lastw = {}
        self.reads = {}
        self.dsems = {}
        self.nsem = len(self.engs)
        self.excl = set()

    def _dsem(self, name):
        if name not in self.dsems:
            self.dsems[name] = [self.nc.alloc_semaphore(f"d_{name}"), 0]
            self.nsem += 1
        return self.dsems[name]

    def op(self, eng, fn, reads=(), writes=(), dsem=None):
        need = {}
        if self.excl:
            writes = list(writes) + [k for k in reads if k in self.excl and k not in writes]

        def req(ev):
            if ev is None:
                return
            sem, val, src = ev
            if src == "pe" and eng == "pe":
                return
            k = id(sem)
            if k not in need or need[k][1] < val:
                need[k] = (sem, val)

        for k in reads:
            req(self.lastw.get(k))
        for k in writes:
            req(self.lastw.get(k))
            for ev in self.reads.get(k, {}).values():
                req(ev)
        waits = []
        wd = self.waited[eng]
        for k, (sem, val) in need.items():
            if wd.get(k, 0) < val:
                wd[k] = val
                waits.append((sem, val))
        if dsem is None:
            if self.cnt[eng] >= SEM_LIMIT:
                self.epoch[eng] += 1
                if not hasattr(self, "old_esems"):
                    self.old_esems = []
                self.old_esems.append((self.esem[eng], self.cnt[eng]))
                self.esem[eng] = self.nc.alloc_semaphore(f"s_{eng}_{self.epoch[eng]}")
                self.cnt[eng] = 0
                self.nsem += 1
            self.cnt[eng] += 1
            ev = (self.esem[eng], self.cnt[eng], eng)
            inc = (self.esem[eng], 1)
        else:
            d = self._dsem(dsem)
            d[1] += 16
            assert d[1] < 60000
            ev = (d[0], d[1], "dma")
            inc = (d[0], 16)
        for k in reads:
            r = self.reads.setdefault(k, {})
            r[id(ev[0])] = ev
        for k in writes:
            self.lastw[k] = ev
            self.reads[k] = {}
        self.ops[eng].append((waits, fn, inc))
        return ev

    def barrier(self):
        evs = [(self.esem[e], self.cnt[e]) for e in self.engs if self.cnt[e] > 0]
        evs += getattr(self, "old_esems", [])
        evs += [(d[0], d[1]) for d in self.dsems.values() if d[1] > 0]
        for e in self.engs:
            wd = self.waited[e]
            waits = []
            for sem, val in evs:
                if wd.get(id(sem), 0) < val:
                    wd[id(sem)] = val
                    waits.append((sem, val))
            self.ops[e].append((waits, None, None))

    def final_wait(self, eng, keys):
        need = {}
        for k in keys:
            ev = self.lastw.get(k)
            if ev is not None:
                kk = id(ev[0])
                if kk not in need or need[kk][1] < ev[1]:
                    need[kk] = (ev[0], ev[1])
        self.ops[eng].append(([(s, v) for s, v in need.values()], None, None))

    def emit(self):
        nc = self.nc
        if os.environ.get("K_VERBOSE"):
            print("nsem", self.nsem, {e: len(self.ops[e]) for e in self.engs}, flush=True)
        with nc.Block() as block:
            def mk(name):
                def body(e):
                    for waits, fn, inc in self.ops[name]:
                        for sem, val in waits:
                            e.wait_ge(sem, val)
                        if fn is not None:
                            ins = fn(e)
                            ins.then_inc(inc[0], inc[1])
                return body
            block.tensor(mk("pe"))
            block.scalar(mk("act"))
            block.vector(mk("dve"))
            block.gpsimd(mk("pool"))
            block.sync(mk("sp"))


class Buf:
    def __init__(self, t, key, base=None):
        self.t = t
        self.key = key
        self.base = base or key

    def __getitem__(self, idx):
        return self.t[idx]


class Ctx:
    def __init__(self, nc, P):
        self.nc = nc
        self.P = P
        self.nbuf = 0
        self.sb_bytes = 16512

    def sb(self, shape, dtype, name=None):
        self.nbuf += 1
        name = name or f"b{self.nbuf}"
        nm = f"{name}_{self.nbuf}"
        esz = 4 if dtype == F32 else 2
        n = 1
        for s in shape[1:]:
            n *= s
        nbytes = (n * esz + 31) // 32 * 32
        off = self.sb_bytes
        self.sb_bytes += nbytes
        self.sb_peak = max(getattr(self, "sb_peak", 0), self.sb_bytes)
        assert self.sb_bytes <= 228000, f"SBUF overflow allocating {nm}: {self.sb_bytes}"
        t = self.nc.alloc_sbuf_tensor_at(nm, list(shape), dtype, offset=off)
        return Buf(t, nm, name)

    def mark(self):
        return self.sb_bytes

    def release(self, m):
        self.P.barrier()
        self.sb_bytes = m

    def ps(self, shape, dtype, name=None):
        self.nbuf += 1
        nm = f"{name or 'ps'}_{self.nbuf}"
        t = self.nc.alloc_psum_tensor(nm, list(shape), dtype)
        self.P.excl.add(nm)
        return Buf(t, nm)

    def dram(self, name, shape, dtype, kind="Internal"):
        t = self.nc.dram_tensor(name, list(shape), dtype, kind=kind)
        return Buf(t.ap(), name)


class Rot:
    def __init__(self, bufs):
        self.bufs = bufs
        self.i = 0

    def next(self):
        b = self.bufs[self.i % len(self.bufs)]
        self.i += 1
        return b


def keys_of(xs):
    out = []
    for x in xs:
        if x is None:
            continue
        if hasattr(x, "key"):
            out.append(x.key)
        else:
            out.append(x)
    return out


def build(debug=None):
    nc = bass.Bass("TRN2", target_bir_lowering=False)
    P = Prog(nc)
    C = Ctx(nc, P)

    def din(name, shape, dt=F32):
        return Buf(nc.dram_tensor(name, list(shape), dt, kind="ExternalInput").ap(), name)

    x_ext = din("x_ext", [NEXT, 1024])
    cT_d = din("cT", [128, 8])
    flags_d = din("flags", [128, 2])
    w_ada_d = din("w_ada", [1024, 6144])
    b_adaT_d = din("b_adaT", [128, 48])
    b_ada_row_d = din("b_ada_row", [1, 6144])
    n1T_d = din("n1T", [128, 8])
    n2T_d = din("n2T", [128, 8])
    w_in_d = din("w_in", [1024, 3592])
    convw_d = din("convw", [128, 12 * 4])
    alog_d = din("alog_bc", [128, 4])
    dtb_d = din("dtb_bc", [128, 4])
    dnw_d = din("dnw_bc", [128, 128])
    w_out_d = din("w_out", [1024, 1024])
    w_router_d = din("w_router", [1024, 32])
    b_router_d = din("b_router_bc", [128, 32])
    if debug in (None, "full", "p4"):
        w1_d = din("w1", [32, 1024, 2048])
        b1T_d = din("b1T", [128, 32 * 16])
        w2_d = din("w2", [32, 1024, 1024])
        b2_d = din("b2", [32, 1024])
    fnw_d = din("fnw_bc", [128, 1024])
    lvlmask_d = din("lvlmask", [128, 14 * 128])
    out_d = Buf(nc.dram_tensor("out", [NMAIN, 1024], F32, kind="ExternalOutput").ap(), "out")

    dbg = debug is not None
    skind = "ExternalOutput" if debug in ("p1", "p2", "p3", "p4a") else "Internal"
    aqT = C.dram("aqT", [512, NMAIN], BF16, skind)
    akT = C.dram("akT", [512, 2048 + NMAIN], BF16, skind)
    avA = C.dram("avA", [2048 + NMAIN, 520], BF16, skind)
    dqT = C.dram("dqT", [512, NMAIN], F32, skind)
    dkT = C.dram("dkT", [512, NEXT], F32, skind)
    dvT = C.dram("dvT", [512, NEXT], F32, skind)
    dzs = C.dram("dzs", [NMAIN, 512], F32, skind)
    mixT = C.dram("mixT", [1024, NMAIN], BF16, skind)
    x2_d = C.dram("x2s", [NMAIN, 1024], F32, skind)
    bg_d = C.dram("bgs", [NEXT, 8], F32, skind)
    if debug == "p3":
        dbg_o = C.dram("dbg_o", [NMAIN, 512], BF16, "ExternalOutput")

    def E(eng, fn, reads=(), writes=(), dsem=None):
        return P.op(eng, fn, keys_of(reads), keys_of(writes), dsem)

    def dma(eng, out_ap, in_ap, reads, writes, dsem):
        E(eng, lambda e: e.dma_start(out=out_ap, in_=in_ap), reads, writes, dsem)

    def mm(out_ap, lhsT, rhs, start, stop, reads, writes):
        E("pe", lambda e: e.matmul(out_ap, lhsT, rhs, start=start, stop=stop), reads, writes)

    def tr(out_ap, in_ap, ident, reads, writes):
        E("pe", lambda e: e.transpose(out_ap, in_ap, ident), reads, writes)

    def act(out_ap, in_ap, func, reads, writes, bias=None, scale=None, accum=None):
        kw = {}
        if bias is not None:
            kw["bias"] = bias
        if scale is not None:
            kw["scale"] = scale
        if accum is not None:
            kw["accum_out"] = accum
        E("act", lambda e: e.activation(out_ap, in_ap, func, **kw), reads, writes)

    def ts(eng, out_ap, in0, s1, s2, op0, op1, reads, writes):
        if op1 is None:
            E(eng, lambda e: e.tensor_scalar(out_ap, in0, s1, None, op0), reads, writes)
        else:
            E(eng, lambda e: e.tensor_scalar(out_ap, in0, s1, s2, op0, op1), reads, writes)

    def tt(eng, out_ap, in0, in1, op, reads, writes):
        E(eng, lambda e: e.tensor_tensor(out_ap, in0, in1, op), reads, writes)

    def stt(eng, out_ap, in0, scalar, in1, op0, op1, reads, writes):
        E(eng, lambda e: e.scalar_tensor_tensor(out_ap, in0, scalar, in1, op0, op1), reads, writes)

    def cp(eng, out_ap, in_ap, reads, writes):
        if eng == "act":
            E("act", lambda e: e.copy(out_ap, in_ap), reads, writes)
        else:
            E(eng, lambda e: e.tensor_copy(out_ap, in_ap), reads, writes)

    def mset(eng, ap, val, writes):
        E(eng, lambda e: e.memset(ap, val), (), writes)

    ident_f = C.sb([128, 128], F32, "identf")
    ident_b = C.sb([128, 128], BF16, "identb")
    ones_b = C.sb([128, 128], BF16, "onesb")
    zero_c = C.sb([128, 1], F32, "zeroc")
    mset("pool", ones_b[:], 1.0, [ones_b])
    mset("pool", zero_c[:], 0.0, [zero_c])
    mset("pool", ident_f[:], 1.0, [ident_f])
    E("pool", lambda e: e.affine_select(ident_f[:], ident_f[:], [[-1, 128]], ALU.is_equal, 0.0,
                                        base=0, channel_multiplier=1), [ident_f], [ident_f])
    cp("dve", ident_b[:], ident_f[:], [ident_f], [ident_b])

    flags = C.sb([128, 2], F32, "flags")
    dma("sp", flags[:], flags_d[:], [], [flags], "c_flags")
    n1T = C.sb([128, 8], F32, "n1T")
    n2T = C.sb([128, 8], F32, "n2T")
    dma("sp", n1T[:], n1T_d[:], [], [n1T], "c_n1")
    dma("sp", n2T[:], n2T_d[:], [], [n2T], "c_n2")
    b_adaT = C.sb([128, 48], F32, "badaT")
    dma("sp", b_adaT[:], b_adaT_d[:], [], [b_adaT], "c_bada")
    cT = C.sb([128, 8], F32, "cT")
    dma("sp", cT[:], cT_d[:], [], [cT], "c_cT")

    psA = [C.ps([128, 512], F32, f"psA{i}") for i in range(6)]
    psT = [C.ps([128, 1024], BF16, f"psT{i}") for i in range(2)]

    modT = C.sb([128, 48], F32, "modT")
    G1bc = C.sb([128, 1024], F32, "G1bc")
    G2bc = C.sb([128, 1024], F32, "G2bc")
    A1T = C.sb([128, 8], F32, "A1T")
    B1T = C.sb([128, 8], F32, "B1T")
    B1pT = C.sb([128, 8], F32, "B1pT")
    A2T = C.sb([128, 8], F32, "A2T")
    B2T = C.sb([128, 8], F32, "B2T")
    convw = C.sb([128, 48], F32, "convw")
    negA = C.sb([128, 4], F32, "negA")
    dtb = C.sb([128, 4], F32, "dtb")
    maskC = C.sb([128, 128], BF16, "maskC")
    maskP = C.sb([128, 128], BF16, "maskP")
    sel65 = C.sb([128, 64], F32, "sel65")
    m_phase = C.mark()

    cond_b = C.sb([128, 8], BF16, "condb")
    cond_rep = C.sb([128, 8, 128], BF16, "condrep")
    act(cond_b[:], cT[:], AF.Silu, [cT], [cond_b])
    for kc in range(8):
        cp("dve", cond_rep[:, kc, :], cond_b[:, kc:kc + 1].to_broadcast([128, 128]), [cond_b], [cond_rep])

    wada = [C.sb([128, 8, 1024], BF16, f"wada{i}") for i in range(2)]
    modps = psA[0]
    w_ada_v = w_ada_d[:].rearrange("(kc p) n -> p kc n", p=128)
    for m in range(6):
        wb = wada[m % 2]
        for kc in range(8):
            dma("pool", wb[:, kc, :], w_ada_v[:, kc, m * 1024:(m + 1) * 1024], [], [wb], f"wada{m % 2}")
        for fc in range(8):
            j = m * 8 + fc
            for kc in range(8):
                mm(modps[:, j:j + 1], wb[:, kc, fc * 128:(fc + 1) * 128], cond_b[:, kc:kc + 1],
                   kc == 0, kc == 7, [wb, cond_b], [modps])
        if m in (2, 5):
            Gbc = G1bc if m == 2 else G2bc
            brow = C.sb([128, 1024], F32, f"brow{m}")
            dma("sp", brow[:], b_ada_row_d[0:1, m * 1024:(m + 1) * 1024].partition_broadcast(128), [], [brow], f"brow{m}")
            for hf in range(2):
                pg = psA[1 + hf]
                for kc in range(8):
                    mm(pg[:], cond_rep[:, kc, :], wb[:, kc, hf * 512:(hf + 1) * 512], kc == 0, kc == 7,
                       [cond_rep, wb], [pg])
                tt("dve", Gbc[:, hf * 512:(hf + 1) * 512], pg[:], brow[:, hf * 512:(hf + 1) * 512], ALU.add,
                   [pg, brow], [Gbc])
    tt("dve", modT[:], modps[:, 0:48], b_adaT[:], ALU.add, [modps, b_adaT], [modT])
    stt("dve", A1T[:], modT[:, 8:16], 1.0, n1T[:], ALU.add, ALU.mult, [modT, n1T], [A1T])
    cp("dve", B1T[:], modT[:, 0:8], [modT], [B1T])
    ts("dve", B1pT[:], modT[:, 0:8], flags[:, 0:1], None, ALU.mult, None, [modT, flags], [B1pT])
    stt("dve", A2T[:], modT[:, 32:40], 1.0, n2T[:], ALU.add, ALU.mult, [modT, n2T], [A2T])
    cp("dve", B2T[:], modT[:, 24:32], [modT], [B2T])

    if debug == "p0":
        dbg_mod = C.dram("dbg_mod", [128, 48], F32, "ExternalOutput")
        dbg_g1 = C.dram("dbg_g1", [128, 1024], F32, "ExternalOutput")
        dma("sp", dbg_mod[:], modT[:], [modT], [dbg_mod], "dbg1")
        dma("sp", dbg_g1[:], G1bc[:], [G1bc], [dbg_g1], "dbg2")
        P.final_wait("sp", ["dbg_mod", "dbg_g1"])
        P.emit()
        return nc


    C.release(m_phase)
    w_in_sb = C.sb([128, 8, 3592], BF16, "w_in")
    w_in_v = w_in_d[:].rearrange("(kc p) n -> p kc n", p=128)
    for kc in range(8):
        dma("pool", w_in_sb[:, kc, :], w_in_v[:, kc, :], [], [w_in_sb], "w_in")
    dma("sp", convw[:], convw_d[:], [], [convw], "c_convw")
    alog = C.sb([128, 4], F32, "alog")
    dma("sp", alog[:], alog_d[:], [], [alog], "c_alog")
    dma("sp", dtb[:], dtb_d[:], [], [dtb], "c_dtb")
    act(negA[:], alog[:], AF.Exp, [alog], [negA])
    ts("dve", negA[:], negA[:], -1.0, None, ALU.mult, None, [negA], [negA])

    def norm_to_hT(xt, hT, col0, AT, BT, xn_rot, st_rot, pT):
        xn = xn_rot.next()
        st = st_rot.next()
        act(xn[:], xt[:], AF.Square, [xt], [xn, st], accum=st[:, 0:1])
        act(st[:, 1:2], st[:, 0:1], AF.Ln, [st], [st], bias=EPS, scale=1.0 / 1024)
        act(st[:, 2:3], st[:, 1:2], AF.Exp, [st], [st], scale=-0.5)
        act(xn[:], xt[:], AF.Copy, [xt, st], [xn], scale=st[:, 2:3])
        for kc in range(8):
            tr(pT[:, kc * 128:(kc + 1) * 128], xn[:, kc * 128:(kc + 1) * 128], ident_b[:], [xn, ident_b], [pT])
        tmp = st_rot_big.next()
        tt("dve", tmp[:].rearrange("p (k t) -> p k t", k=8), pT[:].rearrange("p (k t) -> p k t", k=8),
           AT[:].to_broadcast([128, 8, 128]) if False else AT[:, :, None].to_broadcast([128, 8, 128]), ALU.mult, [pT, AT], [tmp])
        tt("dve", hT[:, :, col0:col0 + 128], tmp[:].rearrange("p (k t) -> p k t", k=8),
           BT[:, :, None].to_broadcast([128, 8, 128]), ALU.add, [tmp, BT], [hT])

    xt_rot = Rot([C.sb([128, 1024], F32, f"xt{i}") for i in range(2)])
    xn_rot = Rot([C.sb([128, 1024], BF16, f"xn{i}") for i in range(2)])
    st_rot = Rot([C.sb([128, 4], F32, f"st{i}") for i in range(2)])
    st_rot_big = Rot([C.sb([128, 1024], F32, f"hTtmp{i}") for i in range(2)])
    hT_rot = Rot([C.sb([128, 8, 512], BF16, f"hT{i}") for i in range(2)])
    raw = [C.sb([128, 515], F32, f"raw{i}") for i in range(12)]
    for r_ in raw:
        mset("pool", r_[:, 0:3], 0.0, [r_])
    cacc_rot = Rot([C.sb([128, 512], F32, f"cacc{i}") for i in range(3)])
    sq_rot = Rot([C.sb([128, 512], BF16, f"sq{i}") for i in range(2)])
    ob_rot = Rot([C.sb([128, 512], BF16, f"ob{i}") for i in range(3)])
    of_rot = Rot([C.sb([128, 512], F32, f"of{i}") for i in range(3)])
    va_rot = Rot([C.sb([128, 8, 65], BF16, f"va{i}") for i in range(2)])
    for vb_ in va_rot.bufs:
        mset("pool", vb_[:], 1.0, [vb_])
    bgt_rot = Rot([C.sb([128, 16], F32, f"bgt{i}") for i in range(2)])
    ps_rot = Rot(psA)
    psT_rot = Rot(psT)
    x_v = x_ext[:].rearrange("(t p) d -> t p d", p=128)
    C_AQ, C_AK, C_AV, C_DQ, C_DK, C_DV, C_DZ, C_DB, C_DA = 0, 512, 1024, 1536, 2048, 2560, 3072, 3584, 3588

    n_groups = 16 if debug is None else int(os.environ.get("K_NGROUPS", "16"))
    hT_of = {}

    def p1_stage_a(g):
        is_pre = g < 8
        hT = hT_rot.next()
        for ti in range(4):
            t = g * 4 + ti
            xt = xt_rot.next()
            dma("sp", xt[:], x_v[t], [], [xt], f"xt{t % 2}")
            norm_to_hT(xt, hT, ti * 128, A1T, B1pT if is_pre else B1T, xn_rot, st_rot, psT_rot.next())
        hT_of[g] = hT

    p1_stage_a(0)
    for g in range(n_groups):
        is_pre = g < 8
        if g + 1 < n_groups:
            p1_stage_a(g + 1)
        hT = hT_of.pop(g)
        tok0 = g * 512
        mtok0 = tok0 - NPRE
        ktok0 = tok0 - 2048

        def proj_fm(col0):
            pp = ps_rot.next()
            for kc in range(8):
                mm(pp[:], w_in_sb[:, kc, col0:col0 + 128], hT[:, kc, :], kc == 0, kc == 7, [w_in_sb, hT], [pp])
            return pp

        if not is_pre:
            for c4 in range(4):
                pp = proj_fm(C_AQ + c4 * 128)
                ob = ob_rot.next()
                act(ob[:], pp[:], AF.Copy, [pp], [ob], scale=0.125)
                dma("pool", aqT[c4 * 128:(c4 + 1) * 128, mtok0:mtok0 + 512], ob[:], [ob], [("aqT", g)], f"st_{ob.base}")
        if g >= 4:
            for c4 in range(4):
                pp = proj_fm(C_AK + c4 * 128)
                ob = ob_rot.next()
                cp("act", ob[:], pp[:], [pp], [ob])
                dma("pool", akT[c4 * 128:(c4 + 1) * 128, ktok0:ktok0 + 512], ob[:], [ob], [("akT", g)], f"st_{ob.base}")
        for cc in range(12):
            if is_pre and cc < 4 and g != 7:
                continue
            pp = proj_fm(C_DQ + cc * 128)
            rw = raw[cc]
            cp("dve", rw[:, 3:515], pp[:], [pp], [rw])
            if is_pre and cc < 4:
                cp("pool", rw[:, 0:3], rw[:, 512:515], [rw], [rw])
                continue
            ca = cacc_rot.next()
            ts("dve", ca[:], rw[:, 3:515], convw[:, cc * 4 + 3:cc * 4 + 4], None, ALU.mult, None, [rw, convw], [ca])
            for j in (2, 1, 0):
                stt("dve", ca[:], rw[:, j:j + 512], convw[:, cc * 4 + j:cc * 4 + j + 1], ca[:], ALU.mult, ALU.add,
                    [rw, convw, ca], [ca])
            cp("pool", rw[:, 0:3], rw[:, 512:515], [rw], [rw])
            of = of_rot.next()
            act(of[:], ca[:], AF.Silu, [ca], [of])
            if cc < 8:
                sq = sq_rot.next()
                act(sq[:], of[:], AF.Square, [of], [sq])
                pn = ps_rot.next()
                mm(pn[:], ones_b[:], sq[:], True, True, [ones_b, sq], [pn])
                rn = cacc_rot.next()
                act(rn[:], pn[:], AF.Ln, [pn], [rn], bias=EPS)
                act(rn[:], rn[:], AF.Exp, [rn], [rn], scale=-0.5)
                sc = (128.0 ** -0.5) if cc < 4 else 1.0
                stt("dve", of[:], of[:], sc, rn[:], ALU.mult, ALU.mult, [of, rn], [of])
            if cc < 4:
                dst = dqT[cc * 128:(cc + 1) * 128, mtok0:mtok0 + 512]
                dk_ = ("dqT", g)
            elif cc < 8:
                dst = dkT[(cc - 4) * 128:(cc - 3) * 128, tok0:tok0 + 512]
                dk_ = ("dkT", g)
            else:
                dst = dvT[(cc - 8) * 128:(cc - 7) * 128, tok0:tok0 + 512]
                dk_ = ("dvT", g)
            dma("pool", dst, of[:], [of], [dk_], f"st_{of.base}")
        for ti in range(4):
            t = g * 4 + ti
            lhs = lambda kc: hT[:, kc, ti * 128:(ti + 1) * 128]
            if g >= 4:
                pp = ps_rot.next()
                for kc in range(8):
                    mm(pp[:], lhs(kc), w_in_sb[:, kc, C_AV:C_AV + 512], kc == 0, kc == 7, [w_in_sb, hT], [pp])
                va = va_rot.next()
                cp("act", va[:, :, 0:64], pp[:].rearrange("p (h e) -> p h e", h=8), [pp], [va])
                dma("pool", avA[ktok0 + ti * 128:ktok0 + (ti + 1) * 128, :], va[:].rearrange("p h e -> p (h e)"),
                    [va], [("avA", t)], f"st_{va.base}")
            if not is_pre:
                pp = ps_rot.next()
                for kc in range(8):
                    mm(pp[:], lhs(kc), w_in_sb[:, kc, C_DZ:C_DZ + 512], kc == 0, kc == 7, [w_in_sb, hT], [pp])
                of = of_rot.next()
                act(of[:], pp[:], AF.Silu, [pp], [of])
                dma("pool", dzs[mtok0 + ti * 128:mtok0 + (ti + 1) * 128, :], of[:], [of], [("dzs", t)], f"st_{of.base}")
            pp = ps_rot.next()
            for kc in range(8):
                mm(pp[:, 0:8], lhs(kc), w_in_sb[:, kc, C_DB:C_DB + 8], kc == 0, kc == 7, [w_in_sb, hT], [pp])
            bgt = bgt_rot.next()
            act(bgt[:, 0:4], pp[:, 0:4], AF.Sigmoid, [pp], [bgt])
            tt("dve", bgt[:, 8:12], pp[:, 4:8], dtb[:], ALU.add, [pp, dtb], [bgt])
            act(bgt[:, 12:16], bgt[:, 8:12], AF.Exp, [bgt], [bgt])
            act(bgt[:, 8:12], bgt[:, 12:16], AF.Ln, [bgt], [bgt], bias=1.0)
            tt("dve", bgt[:, 4:8], bgt[:, 8:12], negA[:], ALU.mult, [bgt, negA], [bgt])
            dma("pool", bg_d[t * 128:(t + 1) * 128, :], bgt[:, 0:8], [bgt], [("bg", t)], f"st_{bgt.base}")

    if debug == "p1":
        P.final_wait("pool", [k for k in P.lastw if isinstance(k, tuple)])
        P.emit()
        return nc

    C.release(m_phase)
    mtmp = C.sb([128, 128], F32, "mtmp")
    mset("pool", mtmp[:], 1.0, [mtmp])
    E("pool", lambda e: e.affine_select(mtmp[:], mtmp[:], [[1, 128]], ALU.is_ge, 0.0, base=0, channel_multiplier=-1),
      [mtmp], [mtmp])
    mtmp2 = C.sb([128, 128], F32, "mtmp2")
    mset("pool", mtmp2[:], 1.0, [mtmp2])
    E("pool", lambda e: e.affine_select(mtmp2[:], mtmp2[:], [[-1, 128]], ALU.is_ge, 0.0, base=0, channel_multiplier=1),
      [mtmp2], [mtmp2])
    mask01 = C.sb([128, 256], BF16, "mask01")
    cp("dve", mask01[:, 0:128], mtmp2[:], [mtmp2], [mask01])
    cp("dve", mask01[:, 128:256], mtmp[:], [mtmp], [mask01])
    mset("pool", sel65[:], 1.0, [sel65])
    E("pool", lambda e: e.affine_select(sel65[:], sel65[:], [[0, 64]], ALU.is_equal, 0.0, base=-64, channel_multiplier=1),
      [sel65], [sel65])

    qs = C.sb([128, 4, 2048], BF16, "qs")
    ks = C.sb([128, 4, 4096], BF16, "ks")
    acc = C.sb([128, 8, 2048], F32, "acc")
    vt_rot = Rot([C.sb([128, 8, 65], BF16, f"vt{i}") for i in range(4)])
    pt_rot = Rot([C.sb([128, 256], BF16, f"pt{i}") for i in range(8)])

    class View2:
        def __init__(self, ap, key):
            self.ap = ap
            self.key = key

        def __getitem__(self, idx):
            return self.ap[idx]

    ps2_rot = Rot(list(psA) + [View2(psT[i][:].bitcast(F32), psT[i].key) for i in range(2)])
    rd_rot = Rot([C.sb([64, 512], F32, f"rd{i}") for i in range(2)])
    mo_rot = Rot([C.sb([64, 512], BF16, f"mo{i}") for i in range(2)])
    n_sb = 2 if debug is None else int(os.environ.get("K_NSB", "2"))
    for sb_ in range(n_sb):
        for p4 in range(4):
            dma("sp", qs[:, p4, :], aqT[p4 * 128:(p4 + 1) * 128, sb_ * 2048:(sb_ + 1) * 2048],
                [("aqT", 8 + sb_ * 4 + i) for i in range(4)], [qs], "qs")
            dma("sp", ks[:, p4, :], akT[p4 * 128:(p4 + 1) * 128, sb_ * 2048:sb_ * 2048 + 4096],
                [("akT", 4 + sb_ * 4 + i) for i in range(8)], [ks], "ks")
        for d in (1, 4, 16):
            nblk = 16 // d
            for r in range(d):
                vts = {}
                for n in range(nblk):
                    for kb in (n - 1, n):
                        if kb in vts:
                            continue
                        idx0 = sb_ * 2048 + 2048 + r + d * 128 * kb
                        vt = vt_rot.next()
                        t_lo = idx0 // 128
                        t_hi = (idx0 + d * 127) // 128
                        dma("sp", vt[:].rearrange("p h e -> p (h e)"), avA[idx0:idx0 + 127 * d + 1:d, :],
                            [("avA", 16 + tt_) for tt_ in range(t_lo, t_hi + 1)], [vt], f"ld_{vt.base}")
                        vts[kb] = vt
                    vprev, vcur = vts[n - 1], vts[n]
                    q0 = r + d * 128 * n
                    kp0 = 2048 + r + d * 128 * (n - 1)
                    kc0 = 2048 + r + d * 128 * n
                    pend = []
                    for h in range(8):
                        p4, r0 = h // 2, (h % 2) * 64
                        qv = qs[r0:r0 + 64, p4, q0:q0 + 127 * d + 1:d]
                        kpv = ks[r0:r0 + 64, p4, kp0:kp0 + 127 * d + 1:d]
                        kcv = ks[r0:r0 + 64, p4, kc0:kc0 + 127 * d + 1:d]
                        pss = ps2_rot.next()
                        mm(pss[:, 0:128], kpv, qv, True, True, [ks, qs], [pss])
                        mm(pss[:, 128:256], kcv, qv, True, True, [ks, qs], [pss])
                        pt = pt_rot.next()
                        if sb_ == 0 and n == 0:
                            act(pt[:, 0:128], pss[:, 0:128], AF.Exp, [pss, flags], [pt], bias=flags[:, 1:2])
                            act(pt[:, 128:256], pss[:, 128:256], AF.Exp, [pss], [pt])
                        else:
                            act(pt[:], pss[:, 0:256], AF.Exp, [pss], [pt])
                        tt("dve", pt[:], pt[:], mask01[:], ALU.mult, [pt, mask01], [pt])

                        def pv_part(h=h, pt=pt):
                            pso = ps2_rot.next()
                            mm(pso[0:65, 0:128], vprev[:, h, :], pt[:, 0:128], True, False, [vprev, pt], [pso])
                            mm(pso[0:65, 0:128], vcur[:, h, :], pt[:, 128:256], False, True, [vcur, pt], [pso])
                            av_ = acc[0:65, h, q0:q0 + 127 * d + 1:d]
                            if d == 1:
                                cp("dve", av_, pso[0:65, 0:128], [pso], [("acc", h)])
                            else:
                                tt("dve", av_, av_, pso[0:65, 0:128], ALU.add, [pso, ("acc", h)], [("acc", h)])
                        pend.append(pv_part)
                        if len(pend) > 2:
                            pend.pop(0)()
                    for f_ in pend:
                        f_()
        for h in range(8):
            for c4 in range(4):
                psd = ps_rot.next()
                mm(psd[0:64, :], sel65[0:65, :], acc[0:65, h, c4 * 512:(c4 + 1) * 512], True, True,
                   [sel65, ("acc", h)], [psd])
                rd = rd_rot.next()
                E("dve", lambda e, rd=rd, psd=psd: e.reciprocal(rd[:], psd[0:64, :]), keys_of([psd]), keys_of([rd]))
                mo = mo_rot.next()
                tt("dve", mo[:], acc[0:64, h, c4 * 512:(c4 + 1) * 512], rd[:], ALU.mult, [rd, ("acc", h)], [mo])
                dma("pool", mixT[h * 64:(h + 1) * 64, sb_ * 2048 + c4 * 512:sb_ * 2048 + (c4 + 1) * 512], mo[:],
                    [mo], [("mixT_a", sb_)], f"st_{mo.base}")

    if debug == "p2":
        P.final_wait("pool", [k for k in P.lastw if isinstance(k, tuple)])
        P.emit()
        return nc

    C.release(m_phase)

    class View:
        def __init__(self, ap, key):
            self.ap = ap
            self.key = key

        def __getitem__(self, idx):
            return self.ap[idx]

    psq = Rot([View(psA[i][:, q * 128:(q + 1) * 128], psA[i].key) for q in range(4) for i in range(6)])
    pst = Rot([View(psT[i][:, q * 128:(q + 1) * 128], psT[i].key) for q in range(4) for i in range(2)])

    Uinc = C.sb([128, 128], F32, "Uinc")
    mset("pool", Uinc[:], 1.0, [Uinc])
    E("pool", lambda e: e.affine_select(Uinc[:], Uinc[:], [[1, 128]], ALU.is_ge, 0.0, base=0, channel_multiplier=-1),
      [Uinc], [Uinc])
    ones_f = C.sb([128, 128], F32, "onesf")
    mset("pool", ones_f[:], 1.0, [ones_f])
    bigS = C.sb([128, 128], F32, "bigS")
    mset("pool", bigS[:], 0.0, [bigS])
    E("pool", lambda e: e.affine_select(bigS[:], bigS[:], [[-1, 128]], ALU.is_ge, 1e4, base=-1, channel_multiplier=1),
      [bigS], [bigS])
    nbigT = C.sb([128, 128], F32, "nbigT")
    mset("pool", nbigT[:], 0.0, [nbigT])
    E("pool", lambda e: e.affine_select(nbigT[:], nbigT[:], [[1, 128]], ALU.is_ge, -1e4, base=0, channel_multiplier=-1),
      [nbigT], [nbigT])
    lm = C.sb([128, 14, 128], BF16, "lm")
    dma("pool", lm[:].rearrange("p a b -> p (a b)"), lvlmask_d[:], [], [lm], "c_lm")
    dnw = C.sb([128, 128], F32, "dnw")
    dma("sp", dnw[:], dnw_d[:], [], [dnw], "c_dnw")
    Sst = [C.sb([128, 128], F32, f"S{h}") for h in range(4)]
    for S_ in Sst:
        mset("pool", S_[:], 0.0, [S_])

    def rot(shape, dt, name, n):
        return Rot([C.sb(shape, dt, f"{name}{i}") for i in range(n)])

    kb_rot = rot([128, 4, 128], BF16, "kb", 3)
    vb4_rot = rot([128, 4, 128], BF16, "vb4_", 3)
    qb_rot = rot([128, 4, 128], BF16, "qb", 3)
    kf_rot = rot([128, 4, 128], F32, "kf", 3)
    vf_rot = rot([128, 4, 128], F32, "vf", 3)
    qf_rot = rot([128, 4, 128], F32, "qf", 3)
    bgt3_rot = rot([128, 8], F32, "bgt3_", 3)
    dzt_rot = rot([128, 512], F32, "dzt", 3)
    gcs_rot = rot([128, 32], F32, "gcs", 3)
    INFLIGHT = int(os.environ.get("K_INFLIGHT", "3"))
    ND = 4 * INFLIGHT
    f32pools = {n: rot([128, 128], F32, n, ND) for n in
                ["grep", "tS", "tT", "DmS", "DmT", "egc", "qdT", "kd", "intraT", "wT", "uu", "vn", "gz"]}
    b16pools = {n: rot([128, 128], BF16, n, ND) for n in
                ["kbg", "vb", "Ab", "ATb", "Zs", "Zs2", "on", "onT"]}
    gh_rings = [rot([128, 128], BF16, f"GH{u}_", 4) for u in range(ND)]
    ul_rings = [rot([128, 128], BF16, f"UL{u}_", 4) for u in range(ND)]
    st3_rot = rot([128, 4], F32, "st3_", ND)
    dk_v = dkT[:].rearrange("(h p) t -> p h t", p=128)
    dv_v = dvT[:].rearrange("(h p) t -> p h t", p=128)
    dq_v = dqT[:].rearrange("(h p) t -> p h t", p=128)

    n_chunks = 64 if debug is None else int(os.environ.get("K_NCHUNKS", "64"))

    def start_chunk(c):
        main = c >= 32
        g_ = c // 4
        Kf4 = kf_rot.next()
        Vf4 = vf_rot.next()
        dma("sp", Kf4[:], dk_v[:, :, c * 128:(c + 1) * 128], [("dkT", g_)], [Kf4], f"ld_{Kf4.base}")
        dma("sp", Vf4[:], dv_v[:, :, c * 128:(c + 1) * 128], [("dvT", g_)], [Vf4], f"ld_{Vf4.base}")
        bgt = bgt3_rot.next()
        dma("sp", bgt[:], bg_d[c * 128:(c + 1) * 128, :], [("bg", c)], [bgt], f"ld_{bgt.base}")
        if main:
            Qf4 = qf_rot.next()
            dma("sp", Qf4[:], dq_v[:, :, (c - 32) * 128:(c - 31) * 128], [("dqT", g_)], [Qf4], f"ld_{Qf4.base}")
            dzt = dzt_rot.next()
            dma("sp", dzt[:], dzs[(c - 32) * 128:(c - 31) * 128, :], [("dzs", c)], [dzt], f"ld_{dzt.base}")
        Kb4 = kb_rot.next()
        Vb4 = vb4_rot.next()
        cp("pool", Kb4[:], Kf4[:], [Kf4], [Kb4])
        cp("pool", Vb4[:], Vf4[:], [Vf4], [Vb4])
        if main:
            Qb4 = qb_rot.next()
            cp("pool", Qb4[:], Qf4[:], [Qf4], [Qb4])
        psg0 = psq.next()
        mm(psg0[:, 0:4], Uinc[:], bgt[:, 4:8], True, True, [Uinc, bgt], [psg0])
        mm(psg0[:, 4:8], ones_f[:], bgt[:, 4:8], True, True, [ones_f, bgt], [psg0])
        gcs = gcs_rot.next()
        cp("act", gcs[:, 0:8], psg0[:, 0:8], [psg0], [gcs])
        act(gcs[:, 20:24], gcs[:, 0:4], AF.Exp, [gcs], [gcs])
        tt("dve", gcs[:, 8:12], gcs[:, 20:24], bgt[:, 0:4], ALU.mult, [gcs, bgt], [gcs])
        tt("dve", gcs[:, 24:28], gcs[:, 4:8], gcs[:, 0:4], ALU.subtract, [gcs], [gcs])
        act(gcs[:, 12:16], gcs[:, 24:28], AF.Exp, [gcs], [gcs])
        act(gcs[:, 16:20], gcs[:, 4:8], AF.Exp, [gcs], [gcs])

        def head_gen(h):
            uslot = (c % INFLIGHT) * 4 + h
            gh_rot = gh_rings[uslot]
            ul_rot = ul_rings[uslot]
            gh_rot.i = 0
            ul_rot.i = 0
            Kf = Kf4[:, h, :]
            Vf = Vf4[:, h, :]
            gcc = gcs[:, h:h + 1]
            beta_c = bgt[:, h:h + 1]
            fp = {n: p_.next() for n, p_ in f32pools.items()}
            bp = {n: p_.next() for n, p_ in b16pools.items()}
            cp("dve", fp["grep"][:], bgt[:, 4 + h:5 + h].to_broadcast([128, 128]), [bgt], [fp["grep"]])
            psg = psq.next()
            mm(psg[:], fp["grep"][:], Uinc[:], True, True, [fp["grep"], Uinc], [psg])
            yield
            _sub = int(os.environ.get("K_P3SUB", "99")) if debug is not None else 99
            if _sub < 1:
                return
            stt("dve", fp["tS"][:], psg[:], gcc, bigS[:], ALU.subtract, ALU.max, [psg, gcs, bigS], [fp["tS"]])
            if _sub < 2:
                return
            act(fp["DmS"][:], fp["tS"][:], AF.Exp, [fp["tS"]], [fp["DmS"]], scale=-1.0)
            if _sub < 3:
                return
            if main:
                stt("dve", fp["tT"][:], psg[:], gcc, nbigT[:], ALU.subtract, ALU.min, [psg, gcs, nbigT], [fp["tT"]])
                act(fp["DmT"][:], fp["tT"][:], AF.Exp, [fp["tT"]], [fp["DmT"]])
                act(fp["egc"][:], psg[:], AF.Exp, [psg], [fp["egc"]])
                tt("pool", fp["qdT"][:], Qf4[:, h, :], fp["egc"][:], ALU.mult, [Qf4, fp["egc"]], [fp["qdT"]])
            Kb = Kb4[:, h, :]
            psk = psq.next()
            mm(psk[:], Kb, ident_b[:], True, True, [Kb4, ident_b], [psk])
            psv = psq.next()
            mm(psv[:], Vb4[:, h, :], ident_b[:], True, True, [Vb4, ident_b], [psv])
            yield
            act(bp["kbg"][:], psk[:], AF.Copy, [psk, gcs], [bp["kbg"]], scale=gcs[:, 8 + h:9 + h])
            act(fp["kd"][:], psk[:], AF.Copy, [psk, gcs], [fp["kd"]], scale=gcs[:, 12 + h:13 + h])
            act(bp["vb"][:], psv[:], AF.Copy, [psv, bgt], [bp["vb"]], scale=beta_c)
            pskk = psq.next()
            mm(pskk[:], Kb, Kb, True, True, [Kb4], [pskk])
            if main:
                psqk = psq.next()
                mm(psqk[:], Kb, Qb4[:, h, :], True, True, [Kb4, Qb4], [psqk])
            yield
            stt("dve", bp["Ab"][:], pskk[:], beta_c, fp["DmS"][:], ALU.mult, ALU.mult, [pskk, bgt, fp["DmS"]], [bp["Ab"]])
            if main:
                tt("dve", fp["intraT"][:], psqk[:], fp["DmT"][:], ALU.mult, [psqk, fp["DmT"]], [fp["intraT"]])
            pt_ = pst.next()
            tr(pt_[:], bp["Ab"][:], ident_b[:], [bp["Ab"], ident_b], [pt_])
            cp("act", bp["ATb"][:], pt_[:], [pt_], [bp["ATb"]])
            yield
            A_, AT_ = bp["Ab"], bp["ATb"]
            G = gh_rot.next()
            H = gh_rot.next()
            tt("dve", G[:], AT_[:], lm[:, 0, :], ALU.mult, [AT_, lm], [G])
            tt("dve", G[:], G[:], ident_b[:], ALU.add, [G, ident_b], [G])
            tt("dve", H[:], A_[:], lm[:, 1, :], ALU.mult, [A_, lm], [H])
            tt("dve", H[:], H[:], ident_b[:], ALU.add, [H, ident_b], [H])
            Ub, Lb = {}, {}

            def mk_masks(k):
                Lb[k] = ul_rot.next()
                tt("pool", Lb[k][:], A_[:], lm[:, 2 * k + 1, :], ALU.mult, [A_, lm], [Lb[k]])
                if k < 6:
                    Ub[k] = ul_rot.next()
                    tt("pool", Ub[k][:], AT_[:], lm[:, 2 * k, :], ALU.mult, [AT_, lm], [Ub[k]])

            mk_masks(1)
            yield
            for k in range(1, 7):
                last = k == 6
                if not last:
                    mk_masks(k + 1)
                psz = psq.next()
                mm(psz[:], Lb[k][:], G[:], True, True, [Lb[k], G], [psz])
                Zs = b16pools["Zs"].next()
                cp("act", Zs[:], psz[:], [psz], [Zs])
                if not last:
                    psz2 = psq.next()
                    mm(psz2[:], Ub[k][:], H[:], True, True, [Ub[k], H], [psz2])
                    Zs2 = b16pools["Zs2"].next()
                    cp("act", Zs2[:], psz2[:], [psz2], [Zs2])
                yield
                psy = psq.next()
                mm(psy[:], H[:], Zs[:], True, True, [H, Zs], [psy])
                Gn = gh_rot.next()
                tt("dve", Gn[:], G[:], psy[:], ALU.subtract, [G, psy], [Gn])
                if not last:
                    psy2 = psq.next()
                    mm(psy2[:], G[:], Zs2[:], True, True, [G, Zs2], [psy2])
                    Hn = gh_rot.next()
                    tt("dve", Hn[:], H[:], psy2[:], ALU.subtract, [H, psy2], [Hn])
                    H = Hn
                else:
                    gh_rot.next()
                G = Gn
                yield
            psw = psq.next()
            mm(psw[:], bp["kbg"][:], G[:], True, True, [bp["kbg"], G], [psw])
            cp("act", fp["wT"][:], psw[:], [psw], [fp["wT"]])
            psu = psq.next()
            mm(psu[:], G[:], bp["vb"][:], True, True, [G, bp["vb"]], [psu])
            cp("act", fp["uu"][:], psu[:], [psu], [fp["uu"]])
            yield
            S_ = Sst[h]
            ps1 = psq.next()
            mm(ps1[:], fp["wT"][:], S_[:], True, True, [fp["wT"], S_], [ps1])
            tt("dve", fp["vn"][:], fp["uu"][:], ps1[:], ALU.subtract, [fp["uu"], ps1], [fp["vn"]])
            yield
            if main:
                pso = psq.next()
                mm(pso[:], fp["qdT"][:], S_[:], True, False, [fp["qdT"], S_], [pso])
                mm(pso[:], fp["intraT"][:], fp["vn"][:], False, True, [fp["intraT"], fp["vn"]], [pso])
            ps2 = psq.next()
            mm(ps2[:], fp["kd"][:], fp["vn"][:], True, True, [fp["kd"], fp["vn"]], [ps2])
            stt("dve", S_[:], S_[:], gcs[:, 16 + h:17 + h], ps2[:], ALU.mult, ALU.add, [S_, gcs, ps2], [S_])
            yield
            if main:
                st = st3_rot.next()
                act(fp["tS"][:], pso[:], AF.Square, [pso], [fp["tS"], st], accum=st[:, 0:1])
                act(st[:, 1:2], st[:, 0:1], AF.Ln, [st], [st], bias=EPS, scale=1.0 / 128)
                act(st[:, 2:3], st[:, 1:2], AF.Exp, [st], [st], scale=-0.5)
                tt("pool", fp["gz"][:], dnw[:], dzt[:, h * 128:(h + 1) * 128], ALU.mult, [dnw, dzt], [fp["gz"]])
                stt("dve", bp["on"][:], pso[:], st[:, 2:3], fp["gz"][:], ALU.mult, ALU.mult, [pso, st, fp["gz"]], [bp["on"]])
                yield
                pt2 = pst.next()
                tr(pt2[:], bp["on"][:], ident_b[:], [bp["on"], ident_b], [pt2])
                cp("act", bp["onT"][:], pt2[:], [pt2], [bp["onT"]])
                m0 = (c - 32) * 128
                dma("pool", mixT[512 + h * 128:512 + (h + 1) * 128, m0:m0 + 128], bp["onT"][:], [bp["onT"]],
                    [("mixT_d", c)], f"st_{bp['onT'].base}")
                if debug == "p3":
                    dma("pool", dbg_o[m0:m0 + 128, h * 128:(h + 1) * 128], bp["on"][:], [bp["on"]], [("dbg_o", c, h)],
                        f"st2_{bp['on'].base}")

        return [head_gen(h) for h in range(4)]

    STAGGER = int(os.environ.get("K_STAGGER", "7"))
    active = []
    next_c = 0
    while next_c < n_chunks or active:
        if next_c < n_chunks and len(active) < INFLIGHT and (not active or active[-1][1] >= STAGGER):
            active.append([start_chunk(next_c), 0])
            next_c += 1
        for ent in active:
            nxt = []
            for g_i in ent[0]:
                try:
                    next(g_i)
                    nxt.append(g_i)
                except StopIteration:
                    pass
            ent[0] = nxt
            ent[1] += 1
        active = [ent for ent in active if ent[0]]

    if debug == "p3":
        P.final_wait("pool", [k for k in P.lastw if isinstance(k, tuple)])
        P.emit()
        return nc

    C.release(m_phase)
    GT = 16
    n_grp = NMAIN // (GT * 128)
    n_exp = 32 if debug is None else int(os.environ.get("K_NEXP", "32"))
    fnw = C.sb([128, 1024], F32, "fnw")
    dma("sp", fnw[:], fnw_d[:], [], [fnw], "c_fnw")
    brt = C.sb([128, 32], F32, "brt")
    dma("sp", brt[:], b_router_d[:], [], [brt], "c_brt")
    wr_sb = C.sb([128, 8, 32], BF16, "wr")
    dma("pool", wr_sb[:], w_router_d[:].rearrange("(kc p) n -> p kc n", p=128), [], [wr_sb], "c_wr")
    ones_row = C.sb([1, 512], BF16, "onesrow")
    mset("pool", ones_row[:], 1.0, [ones_row])
    h2T = C.sb([128, 8, GT * 128], BF16, "h2T")
    Yacc = C.sb([128, GT, 1024], F32, "Yacc")
    gates = C.sb([128, GT, 32], F32, "gates")
    if debug != "p4a":
        b1T = C.sb([128, 512], F32, "b1T")
        dma("sp", b1T[:], b1T_d[:], [], [b1T], "c_b1T")
        b2_sb = C.sb([32, 1024], F32, "b2sb")
        dma("sp", b2_sb[:], b2_d[:], [], [b2_sb], "c_b2")
    m_p4 = C.mark()
    x_main = x_ext[NPRE:NEXT, :].rearrange("(t p) d -> t p d", p=128)
    x2_v = x2_d[:].rearrange("(t p) d -> t p d", p=128)
    out_v = out_d[:].rearrange("(t p) d -> t p d", p=128)
    mix_v = mixT[:].rearrange("(kc p) t -> p kc t", p=128)

    for grp in range(n_grp):
        C.release(m_p4)
        w_out_sb = C.sb([128, 8, 1024], BF16, "w_out")
        for kc in range(8):
            dma("pool", w_out_sb[:, kc, :], w_out_d[kc * 128:(kc + 1) * 128, :], [], [w_out_sb], "w_out")
        mx_rot = rot([128, 8, 128], BF16, "mx", 2)
        xt_rot = rot([128, 1024], F32, "xt4_", 2)
        x2_rot = rot([128, 1024], F32, "x2t", 2)
        xn_rot = rot([128, 1024], BF16, "xn4_", 2)
        st_rot = rot([128, 4], F32, "st4_", 2)
        st_rot_big = rot([128, 1024], F32, "hTtmp4_", 2)
        lg_rot = rot([128, 32], F32, "lg", 2)
        m8_rot = rot([128, 16], F32, "m8", 2)
        for T in range(GT):
            t = grp * GT + T
            tok0 = t * 128
            mx = mx_rot.next()
            dma("sp", mx[:], mix_v[:, :, tok0:tok0 + 128],
                [("mixT_a", tok0 // 2048)] + [("mixT_d", 32 + t)], [mx], f"ld_{mx.base}")
            xt = xt_rot.next()
            dma("sp", xt[:], x_main[t], [], [xt], f"ld_{xt.base}")
            x2t = x2_rot.next()
            for nh in range(2):
                py = ps_rot.next()
                for kc in range(8):
                    mm(py[:], mx[:, kc, :], w_out_sb[:, kc, nh * 512:(nh + 1) * 512], kc == 0, kc == 7,
                       [mx, w_out_sb], [py])
                tt("dve", x2t[:, nh * 512:(nh + 1) * 512], py[:], G1bc[:, nh * 512:(nh + 1) * 512], ALU.mult,
                   [py, G1bc], [x2t])
            tt("pool", x2t[:], x2t[:], xt[:], ALU.add, [x2t, xt], [x2t])
            dma("pool", x2_v[t], x2t[:], [x2t], [("x2", t)], f"st_{x2t.base}")
            norm_to_hT(x2t, h2T, T * 128, A2T, B2T, xn_rot, st_rot, psT_rot.next())
            pl = ps_rot.next()
            for kc in range(8):
                mm(pl[:, 0:32], h2T[:, kc, T * 128:(T + 1) * 128], wr_sb[:, kc, :], kc == 0, kc == 7, [h2T, wr_sb], [pl])
            lg = lg_rot.next()
            m8 = m8_rot.next()
            tt("dve", lg[:], pl[:, 0:32], brt[:], ALU.add, [pl, brt], [lg])
            E("dve", lambda e, m8=m8, lg=lg: e.max(m8[:, 0:8], lg[:]), keys_of([lg]), keys_of([m8]))
            ts("dve", m8[:, 8:9], m8[:, 0:1], -1.0, None, ALU.mult, None, [m8], [m8])
            gt_ = gates[:, T, :]
            act(gt_, lg[:], AF.Exp, [lg, m8], [("gates", T)], bias=m8[:, 8:9])
            ts("dve", lg[:], lg[:], m8[:, 3:4], None, ALU.is_ge, None, [lg, m8], [lg])
            tt("dve", gt_, gt_, lg[:], ALU.mult, [lg, ("gates", T)], [("gates", T)])
            E("dve", lambda e, m8=m8, gt_=gt_: e.reduce_sum(m8[:, 9:10], gt_, AX.X), keys_of([("gates", T)]), keys_of([m8]))
            E("dve", lambda e, m8=m8: e.reciprocal(m8[:, 10:11], m8[:, 9:10]), keys_of([m8]), keys_of([m8]))
            ts("dve", gt_, gt_, m8[:, 10:11], None, ALU.mult, None, [m8, ("gates", T)], [("gates", T)])
        if debug == "p4a":
            dbg_g = C.dram(f"dbg_gates{grp}", [128, GT * 32], F32, "ExternalOutput")
            dma("sp", dbg_g[:], gates[:].rearrange("p t e -> p (t e)"), [("gates", T_) for T_ in range(GT)], [dbg_g.key], "dbgg")
            continue
        C.release(m_p4)
        w1g_rot = rot([128, 8, 512], BF16, "w1g", 2)
        w1u_rot = rot([128, 8, 512], BF16, "w1u", 2)
        w2h_rot = rot([128, 4, 1024], BF16, "w2h", 2)
        gT_rot = rot([32, 128], F32, "gT", 2)
        for T in range(GT):
            pgt = ps_rot.next()
            mm(pgt[0:32, 0:128], gates[:, T, :], ident_f[:], True, True, [("gates", T), ident_f], [pgt])
            gT = gT_rot.next()
            cp("act", gT[:], pgt[0:32, 0:128], [pgt], [gT])
            for nh in range(2):
                pb = ps_rot.next()
                mm(pb[:], gT[:], b2_sb[:, nh * 512:(nh + 1) * 512], True, True, [gT, b2_sb], [pb])
                cp("act", Yacc[:, T, nh * 512:(nh + 1) * 512], pb[:], [pb], [("Yacc", T, nh)])
        actT_rot = rot([128, 4, 512], BF16, "actT", 2)
        g_rot = rot([128, 512], F32, "gg", 2)
        s_rot = rot([128, 512], F32, "ss", 2)
        u_rot = rot([128, 512], F32, "uu4_", 2)
        pend_y = None
        ps8_rot = Rot(list(psA) + [View(psT[i][:].bitcast(F32), psT[i].key) for i in range(2)])
        for e_ in range(n_exp):
            for hf in range(2):
                w1g = w1g_rot.next()
                w1u = w1u_rot.next()
                w2h = w2h_rot.next()
                for kc in range(8):
                    dma("pool", w1g[:, kc, :], w1_d[e_, kc * 128:(kc + 1) * 128, hf * 512:(hf + 1) * 512], [], [w1g], f"ld_{w1g.base}")
                    dma("pool", w1u[:, kc, :], w1_d[e_, kc * 128:(kc + 1) * 128, 1024 + hf * 512:1024 + (hf + 1) * 512], [], [w1u], f"ld_{w1u.base}")
                for fc in range(4):
                    dma("pool", w2h[:, fc, :], w2_d[e_, hf * 512 + fc * 128:hf * 512 + (fc + 1) * 128, :], [], [w2h], f"ld_{w2h.base}")
                for sub in range(GT // 4):
                    actT = actT_rot.next()
                    cs = slice(sub * 512, (sub + 1) * 512)
                    for fc in range(4):
                        pg = ps8_rot.next()
                        for kc in range(8):
                            mm(pg[:], w1g[:, kc, fc * 128:(fc + 1) * 128], h2T[:, kc, cs], kc == 0, kc == 7, [w1g, h2T], [pg])
                        pu = ps8_rot.next()
                        for kc in range(8):
                            mm(pu[:], w1u[:, kc, fc * 128:(fc + 1) * 128], h2T[:, kc, cs], kc == 0, kc == 7, [w1u, h2T], [pu])
                        gg = g_rot.next()
                        ss_ = s_rot.next()
                        uu_ = u_rot.next()
                        bg_col = b1T[:, e_ * 16 + hf * 4 + fc:e_ * 16 + hf * 4 + fc + 1]
                        bu_col = b1T[:, e_ * 16 + 8 + hf * 4 + fc:e_ * 16 + 8 + hf * 4 + fc + 1]
                        ts("dve", gg[:], pg[:], bg_col, 7.0, ALU.add, ALU.min, [pg, b1T], [gg])
                        act(ss_[:], gg[:], AF.Sigmoid, [gg], [ss_], scale=1.702)
                        ts("dve", uu_[:], pu[:], bu_col, 7.0, ALU.add, ALU.min, [pu, b1T], [uu_])
                        ts("dve", uu_[:], uu_[:], -7.0, 1.0, ALU.max, ALU.add, [uu_], [uu_])
                        tt("dve", gg[:], gg[:], ss_[:], ALU.mult, [gg, ss_], [gg])
                        tt("dve", actT[:, fc, :], uu_[:], gg[:], ALU.mult, [uu_, gg], [actT])
                        if fc == 0 and pend_y is not None:
                            pend_y()
                            pend_y = None

                    def y_part(e_=e_, hf=hf, sub=sub, actT=actT, w2h=w2h):
                        for ti in range(4):
                            T = sub * 4 + ti
                            for nh in range(2):
                                py = ps8_rot.next()
                                for fc in range(4):
                                    mm(py[:], actT[:, fc, ti * 128:(ti + 1) * 128], w2h[:, fc, nh * 512:(nh + 1) * 512],
                                       fc == 0, fc == 3, [actT, w2h], [py])
                                ya = Yacc[:, T, nh * 512:(nh + 1) * 512]
                                gcol = gates[:, T, e_:e_ + 1]
                                stt("dve", ya, py[:], gcol, ya, ALU.mult, ALU.add, [py, ("gates", T), ("Yacc", T, nh)], [("Yacc", T, nh)])
                    pend_y = y_part
        if pend_y is not None:
            pend_y()
            pend_y = None
        C.release(m_p4)
        x2r_rot = rot([128, 1024], F32, "x2r", 2)
        x3_rot = rot([128, 1024], F32, "x3_", 2)
        ot_rot = rot([128, 1024], F32, "ot", 2)
        jk_rot = rot([128, 1024], BF16, "jk", 2)
        st5_rot = rot([128, 4], F32, "st5_", 2)
        for T in range(GT):
            t = grp * GT + T
            x2r = x2r_rot.next()
            dma("sp", x2r[:], x2_v[t], [("x2", t)], [x2r], f"ld_{x2r.base}")
            x3 = x3_rot.next()
            tt("pool", x3[:], Yacc[:, T, :], G2bc[:], ALU.mult, [("Yacc", T, 0), ("Yacc", T, 1), G2bc], [x3])
            tt("dve", x3[:], x3[:], x2r[:], ALU.add, [x3, x2r], [x3])
            st = st5_rot.next()
            jk = jk_rot.next()
            act(jk[:], x3[:], AF.Square, [x3], [jk, st], accum=st[:, 0:1])
            act(st[:, 1:2], st[:, 0:1], AF.Ln, [st], [st], bias=EPS, scale=1.0 / 1024)
            act(st[:, 2:3], st[:, 1:2], AF.Exp, [st], [st], scale=-0.5)
            ot = ot_rot.next()
            stt("dve", ot[:], x3[:], st[:, 2:3], fnw[:], ALU.mult, ALU.mult, [x3, st, fnw], [ot])
            dma("pool", out_v[t], ot[:], [ot], ["out"], f"st_{ot.base}")

    if debug == "p4a":
        P.final_wait("sp", [k for k in P.lastw if isinstance(k, tuple)] + [f"dbg_gates{g_}" for g_ in range(n_grp)])
        P.emit()
        return nc
    P.final_wait("sp", ["out"])
    P.emit()
    return nc


def level_masks():
    out = np.zeros((128, 14, 128), np.float32)
    j = np.arange(128)[:, None]
    c = np.arange(128)[None, :]
    for k in range(7):
        b = 1 << k
        mU = ((j // (2 * b)) == (c // (2 * b))) & ((j % (2 * b)) < b) & ((c % (2 * b)) >= b)
        sgn = -1.0 if k == 0 else 1.0
        out[:, 2 * k, :] = sgn * mU
        out[:, 2 * k + 1, :] = sgn * mU.T
    return np.ascontiguousarray(out.reshape(128, 14 * 128))


def make_in_maps(inputs):
    x = np.asarray(inputs["x"], np.float32)
    c = np.asarray(inputs["c"], np.float32)
    f32 = lambda a: np.ascontiguousarray(np.asarray(a, np.float32))

    def colT(v):
        return f32(np.asarray(v).reshape(-1, 128).T)

    def bc(v, n=128):
        v = np.asarray(v, np.float32).reshape(1, -1)
        return f32(np.broadcast_to(v, (n, v.shape[1])))

    shared = {
        "w_ada": f32(inputs["w_ada"][0]),
        "b_adaT": colT(inputs["b_ada"][0]),
        "b_ada_row": f32(inputs["b_ada"][0].reshape(1, -1)),
        "n1T": colT(inputs["norm1_w"][0]),
        "n2T": colT(inputs["norm2_w"][0]),
        "w_in": f32(inputs["w_in"][0]),
        "convw": f32(np.asarray(inputs["conv_w"][0]).reshape(4, 12, 128).transpose(2, 1, 0).reshape(128, 48)),
        "alog_bc": bc(inputs["A_log"][0]),
        "dtb_bc": bc(inputs["dt_bias"][0]),
        "dnw_bc": bc(inputs["dn_norm_w"][0]),
        "w_out": f32(inputs["w_out"][0]),
        "w_router": f32(inputs["w_router"][0]),
        "b_router_bc": bc(inputs["b_router"][0]),
        "w1": f32(inputs["w1"][0]),
        "b1T": f32(np.asarray(inputs["b1"][0]).reshape(32, 16, 128).transpose(2, 0, 1).reshape(128, 512)),
        "w2": f32(inputs["w2"][0]),
        "b2": f32(inputs["b2"][0]),
        "fnw_bc": bc(inputs["final_norm_w"]),
        "lvlmask": level_masks(),
    }
    maps = []
    for core in range(8):
        b, half = core // 2, core % 2
        xe = np.zeros((NEXT, 1024), np.float32)
        if half == 0:
            xe[NPRE:] = x[b, :NMAIN]
        else:
            xe[:] = x[b]
        fl = np.zeros((128, 2), np.float32)
        fl[:, 0] = 1.0 if half == 1 else 0.0
        fl[:, 1] = 0.0 if half == 1 else NEG
        m = dict(shared)
        m["x_ext"] = xe
        m["cT"] = colT(c[b])
        m["flags"] = fl
        maps.append(m)
    return maps


def kernel(**inputs):
    nc = build()
    maps = make_in_maps(inputs)
    res = run_bass_kernel_spmd(nc, maps, core_ids=list(range(8)))
    out = np.zeros((4, 8192, 1024), np.float32)
    for core in range(8):
        b, half = core // 2, core % 2
        out[b, half * NMAIN:(half + 1) * NMAIN] = res.results[core]["out"]
    return out
```

```python
import os
import numpy as np
import concourse.bass as bass
import concourse.mybir as mybir
from concourse.bass_utils import run_bass_kernel_spmd

F32 = mybir.dt.float32
BF16 = mybir.dt.bfloat16
AF = mybir.ActivationFunctionType
ALU = mybir.AluOpType
AX = mybir.AxisListType

NPRE = 4096
NMAIN = 4096
NEXT = NPRE + NMAIN
EPS = 1e-6
NEG = -30000.0
SEM_LIMIT = int(os.environ.get("K_SEMLIM", "8000"))


class Prog:
    def __init__(self, nc):
        self.nc = nc
        self.engs = ["pe", "act", "dve", "pool", "sp"]
        self.ops = {e: [] for e in self.engs}
        self.cnt = {e: 0 for e in self.engs}
        self.epoch = {e: 0 for e in self.engs}
        self.esem = {e: nc.alloc_semaphore(f"s_{e}_0") for e in self.engs}
        self.waited = {e: {} for e in self.engs}
        self.lastw = {}
        self.reads = {}
        self.dsems = {}
        self.nsem = len(self.engs)
        self.excl = set()

    def _dsem(self, name):
        if name not in self.dsems:
            self.dsems[name] = [self.nc.alloc_semaphore(f"d_{name}"), 0]
            self.nsem += 1
        return self.dsems[name]

    def op(self, eng, fn, reads=(), writes=(), dsem=None):
        need = {}
        if self.excl:
            writes = list(writes) + [k for k in reads if k in self.excl and k not in writes]

        def req(ev):
            if ev is None:
                return
            sem, val, src = ev
            if src == "pe" and eng == "pe":
                return
            k = id(sem)
            if k not in need or need[k][1] < val:
                need[k] = (sem, val)

        for k in reads:
            req(self.lastw.get(k))
        for k in writes:
            req(self.lastw.get(k))
            for ev in self.reads.get(k, {}).values():
                req(ev)
        waits = []
        wd = self.waited[eng]
        for k, (sem, val) in need.items():
            if wd.get(k, 0) < val:
                wd[k] = val
                waits.append((sem, val))
        if dsem is None:
            if self.cnt[eng] >= SEM_LIMIT:
                self.epoch[eng] += 1
                if not hasattr(self, "old_esems"):
                    self.old_esems = []
                self.old_esems.append((self.esem[eng], self.cnt[eng]))
                self.esem[eng] = self.nc.alloc_semaphore(f"s_{eng}_{self.epoch[eng]}")
                self.cnt[eng] = 0
                self.nsem += 1
            self.cnt[eng] += 1
            ev = (self.esem[eng], self.cnt[eng], eng)
            inc = (self.esem[eng], 1)
        else:
            d = self._dsem(dsem)
            d[1] += 16
            assert d[1] < 60000
            ev = (d[0], d[1], "dma")
            inc = (d[0], 16)
        for k in reads:
            r = self.reads.setdefault(k, {})
            r[id(ev[0])] = ev
        for k in writes:
            self.lastw[k] = ev
            self.reads[k] = {}
        self.ops[eng].append((waits, fn, inc))
        return ev

    def barrier(self):
        evs = [(self.esem[e], self.cnt[e]) for e in self.engs if self.cnt[e] > 0]
        evs += getattr(self, "old_esems", [])
        evs += [(d[0], d[1]) for d in self.dsems.values() if d[1] > 0]
        for e in self.engs:
            wd = self.waited[e]
            waits = []
            for sem, val in evs:
                if wd.get(id(sem), 0) < val:
                    wd[id(sem)] = val
                    waits.append((sem, val))
            self.ops[e].append((waits, None, None))

    def final_wait(self, eng, keys):
        need = {}
        for k in keys:
            ev = self.lastw.get(k)
            if ev is not None:
                kk = id(ev[0])
                if kk not in need or need[kk][1] < ev[1]:
                    need[kk] = (ev[0], ev[1])
        self.ops[eng].append(([(s, v) for s, v in need.values()], None, None))

    def emit(self):
        nc = self.nc
        if os.environ.get("K_VERBOSE"):
            print("nsem", self.nsem, {e: len(self.ops[e]) for e in self.engs}, flush=True)
        with nc.Block() as block:
            def mk(name):
                def body(e):
                    for waits, fn, inc in self.ops[name]:
                        for sem, val in waits:
                            e.wait_ge(sem, val)
                        if fn is not None:
                            ins = fn(e)
                            ins.then_inc(inc[0], inc[1])
                return body
            block.tensor(mk("pe"))
            block.scalar(mk("act"))
            block.vector(mk("dve"))
            block.gpsimd(mk("pool"))
            block.sync(mk("sp"))


class Buf:
    def __init__(self, t, key, base=None):
        self.t = t
        self.key = key
        self.base = base or key

    def __getitem__(self, idx):
        return self.t[idx]


class Ctx:
    def __init__(self, nc, P):
        self.nc = nc
        self.P = P
        self.nbuf = 0
        self.sb_bytes = 16512

    def sb(self, shape, dtype, name=None):
        self.nbuf += 1
        name = name or f"b{self.nbuf}"
        nm = f"{name}_{self.nbuf}"
        esz = 4 if dtype == F32 else 2
        n = 1
        for s in shape[1:]:
            n *= s
        nbytes = (n * esz + 31) // 32 * 32
        off = self.sb_bytes
        self.sb_bytes += nbytes
        self.sb_peak = max(getattr(self, "sb_peak", 0), self.sb_bytes)
        assert self.sb_bytes <= 228000, f"SBUF overflow allocating {nm}: {self.sb_bytes}"
        t = self.nc.alloc_sbuf_tensor_at(nm, list(shape), dtype, offset=off)
        return Buf(t, nm, name)

    def mark(self):
        return self.sb_bytes

    def release(self, m):
        self.P.barrier()
        self.sb_bytes = m

    def ps(self, shape, dtype, name=None):
        self.nbuf += 1
        nm = f"{name or 'ps'}_{self.nbuf}"
        t = self.nc.alloc_psum_tensor(nm, list(shape), dtype)
        self.P.excl.add(nm)
        return Buf(t, nm)

    def dram(self, name, shape, dtype, kind="Internal"):
        t = self.nc.dram_tensor(name, list(shape), dtype, kind=kind)
        return Buf(t.ap(), name)


class Rot:
    def __init__(self, bufs):
        self.bufs = bufs
        self.i = 0

    def next(self):
        b = self.bufs[self.i % len(self.bufs)]
        self.i += 1
        return b


def keys_of(xs):
    out = []
    for x in xs:
        if x is None:
            continue
        if hasattr(x, "key"):
            out.append(x.key)
        else:
            out.append(x)
    return out


def build(debug=None):
    nc = bass.Bass("TRN2", target_bir_lowering=False)
    P = Prog(nc)
    C = Ctx(nc, P)

    def din(name, shape, dt=F32):
        return Buf(nc.dram_tensor(name, list(shape), dt, kind="ExternalInput").ap(), name)

    x_ext = din("x_ext", [NEXT, 1024])
    cT_d = din("cT", [128, 8])
    flags_d = din("flags", [128, 2])
    w_ada_d = din("w_ada", [1024, 6144])
    b_adaT_d = din("b_adaT", [128, 48])
    b_ada_row_d = din("b_ada_row", [1, 6144])
    n1T_d = din("n1T", [128, 8])
    n2T_d = din("n2T", [128, 8])
    w_in_d = din("w_in", [1024, 3592])
    convw_d = din("convw", [128, 12 * 4])
    alog_d = din("alog_bc", [128, 4])
    dtb_d = din("dtb_bc", [128, 4])
    dnw_d = din("dnw_bc", [128, 128])
    w_out_d = din("w_out", [1024, 1024])
    w_router_d = din("w_router", [1024, 32])
    b_router_d = din("b_router_bc", [128, 32])
    if debug in (None, "full", "p4"):
        w1_d = din("w1", [32, 1024, 2048])
        b1T_d = din("b1T", [128, 32 * 16])
        w2_d = din("w2", [32, 1024, 1024])
        b2_d = din("b2", [32, 1024])
    fnw_d = din("fnw_bc", [128, 1024])
    lvlmask_d = din("lvlmask", [128, 14 * 128])
    out_d = Buf(nc.dram_tensor("out", [NMAIN, 1024], F32, kind="ExternalOutput").ap(), "out")

    dbg = debug is not None
    skind = "ExternalOutput" if debug in ("p1", "p2", "p3", "p4a") else "Internal"
    aqT = C.dram("aqT", [512, NMAIN], BF16, skind)
    akT = C.dram("akT", [512, 2048 + NMAIN], BF16, skind)
    avA = C.dram("avA", [2048 + NMAIN, 520], BF16, skind)
    dqT = C.dram("dqT", [512, NMAIN], F32, skind)
    dkT = C.dram("dkT", [512, NEXT], F32, skind)
    dvT = C.dram("dvT", [512, NEXT], F32, skind)
    dzs = C.dram("dzs", [NMAIN, 512], F32, skind)
    mixT = C.dram("mixT", [1024, NMAIN], BF16, skind)
    x2_d = C.dram("x2s", [NMAIN, 1024], F32, skind)
    bg_d = C.dram("bgs", [NEXT, 8], F32, skind)
    if debug == "p3":
        dbg_o = C.dram("dbg_o", [NMAIN, 512], BF16, "ExternalOutput")

    def E(eng, fn, reads=(), writes=(), dsem=None):
        return P.op(eng, fn, keys_of(reads), keys_of(writes), dsem)

    def dma(eng, out_ap, in_ap, reads, writes, dsem):
        E(eng, lambda e: e.dma_start(out=out_ap, in_=in_ap), reads, writes, dsem)

    def mm(out_ap, lhsT, rhs, start, stop, reads, writes):
        E("pe", lambda e: e.matmul(out_ap, lhsT, rhs, start=start, stop=stop), reads, writes)

    def tr(out_ap, in_ap, ident, reads, writes):
        E("pe", lambda e: e.transpose(out_ap, in_ap, ident), reads, writes)

    def act(out_ap, in_ap, func, reads, writes, bias=None, scale=None, accum=None):
        kw = {}
        if bias is not None:
            kw["bias"] = bias
        if scale is not None:
            kw["scale"] = scale
        if accum is not None:
            kw["accum_out"] = accum
        E("act", lambda e: e.activation(out_ap, in_ap, func, **kw), reads, writes)

    def ts(eng, out_ap, in0, s1, s2, op0, op1, reads, writes):
        if op1 is None:
            E(eng, lambda e: e.tensor_scalar(out_ap, in0, s1, None, op0), reads, writes)
        else:
            E(eng, lambda e: e.tensor_scalar(out_ap, in0, s1, s2, op0, op1), reads, writes)

    def tt(eng, out_ap, in0, in1, op, reads, writes):
        E(eng, lambda e: e.tensor_tensor(out_ap, in0, in1, op), reads, writes)

    def stt(eng, out_ap, in0, scalar, in1, op0, op1, reads, writes):
        E(eng, lambda e: e.scalar_tensor_tensor(out_ap, in0, scalar, in1, op0, op1), reads, writes)

    def cp(eng, out_ap, in_ap, reads, writes):
        if eng == "act":
            E("act", lambda e: e.copy(out_ap, in_ap), reads, writes)
        else:
            E(eng, lambda e: e.tensor_copy(out_ap, in_ap), reads, writes)

    def mset(eng, ap, val, writes):
        E(eng, lambda e: e.memset(ap, val), (), writes)

    ident_f = C.sb([128, 128], F32, "identf")
    ident_b = C.sb([128, 128], BF16, "identb")
    ones_b = C.sb([128, 128], BF16, "onesb")
    zero_c = C.sb([128, 1], F32, "zeroc")
    mset("pool", ones_b[:], 1.0, [ones_b])
    mset("pool", zero_c[:], 0.0, [zero_c])
    mset("pool", ident_f[:], 1.0, [ident_f])
    E("pool", lambda e: e.affine_select(ident_f[:], ident_f[:], [[-1, 128]], ALU.is_equal, 0.0,
                                        base=0, channel_multiplier=1), [ident_f], [ident_f])
    cp("dve", ident_b[:], ident_f[:], [ident_f], [ident_b])

    flags = C.sb([128, 2], F32, "flags")
    dma("sp", flags[:], flags_d[:], [], [flags], "c_flags")
    n1T = C.sb([128, 8], F32, "n1T")
    n2T = C.sb([128, 8], F32, "n2T")
    dma("sp", n1T[:], n1T_d[:], [], [n1T], "c_n1")
    dma("sp", n2T[:], n2T_d[:], [], [n2T], "c_n2")
    b_adaT = C.sb([128, 48], F32, "badaT")
    dma("sp", b_adaT[:], b_adaT_d[:], [], [b_adaT], "c_bada")
    cT = C.sb([128, 8], F32, "cT")
    dma("sp", cT[:], cT_d[:], [], [cT], "c_cT")

    psA = [C.ps([128, 512], F32, f"psA{i}") for i in range(6)]
    psT = [C.ps([128, 1024], BF16, f"psT{i}") for i in range(2)]

    modT = C.sb([128, 48], F32, "modT")
    G1bc = C.sb([128, 1024], F32, "G1bc")
    G2bc = C.sb([128, 1024], F32, "G2bc")
    A1T = C.sb([128, 8], F32, "A1T")
    B1T = C.sb([128, 8], F32, "B1T")
    B1pT = C.sb([128, 8], F32, "B1pT")
    A2T = C.sb([128, 8], F32, "A2T")
    B2T = C.sb([128, 8], F32, "B2T")
    convw = C.sb([128, 48], F32, "convw")
    negA = C.sb([128, 4], F32, "negA")
    dtb = C.sb([128, 4], F32, "dtb")
    maskC = C.sb([128, 128], BF16, "maskC")
    maskP = C.sb([128, 128], BF16, "maskP")
    sel65 = C.sb([128, 64], F32, "sel65")
    m_phase = C.mark()

    cond_b = C.sb([128, 8], BF16, "condb")
    cond_rep = C.sb([128, 8, 128], BF16, "condrep")
    act(cond_b[:], cT[:], AF.Silu, [cT], [cond_b])
    for kc in range(8):
        cp("dve", cond_rep[:, kc, :], cond_b[:, kc:kc + 1].to_broadcast([128, 128]), [cond_b], [cond_rep])

    wada = [C.sb([128, 8, 1024], BF16, f"wada{i}") for i in range(2)]
    modps = psA[0]
    w_ada_v = w_ada_d[:].rearrange("(kc p) n -> p kc n", p=128)
    for m in range(6):
        wb = wada[m % 2]
        for kc in range(8):
            dma("pool", wb[:, kc, :], w_ada_v[:, kc, m * 1024:(m + 1) * 1024], [], [wb], f"wada{m % 2}")
        for fc in range(8):
            j = m * 8 + fc
            for kc in range(8):
                mm(modps[:, j:j + 1], wb[:, kc, fc * 128:(fc + 1) * 128], cond_b[:, kc:kc + 1],
                   kc == 0, kc == 7, [wb, cond_b], [modps])
        if m in (2, 5):
            Gbc = G1bc if m == 2 else G2bc
            brow = C.sb([128, 1024], F32, f"brow{m}")
            dma("sp", brow[:], b_ada_row_d[0:1, m * 1024:(m + 1) * 1024].partition_broadcast(128), [], [brow], f"brow{m}")
            for hf in range(2):
                pg = psA[1 + hf]
                for kc in range(8):
                    mm(pg[:], cond_rep[:, kc, :], wb[:, kc, hf * 512:(hf + 1) * 512], kc == 0, kc == 7,
                       [cond_rep, wb], [pg])
                tt("dve", Gbc[:, hf * 512:(hf + 1) * 512], pg[:], brow[:, hf * 512:(hf + 1) * 512], ALU.add,
                   [pg, brow], [Gbc])
    tt("dve", modT[:], modps[:, 0:48], b_adaT[:], ALU.add, [modps, b_adaT], [modT])
    stt("dve", A1T[:], modT[:, 8:16], 1.0, n1T[:], ALU.add, ALU.mult, [modT, n1T], [A1T])
    cp("dve", B1T[:], modT[:, 0:8], [modT], [B1T])
    ts("dve", B1pT[:], modT[:, 0:8], flags[:, 0:1], None, ALU.mult, None, [modT, flags], [B1pT])
    stt("dve", A2T[:], modT[:, 32:40], 1.0, n2T[:], ALU.add, ALU.mult, [modT, n2T], [A2T])
    cp("dve", B2T[:], modT[:, 24:32], [modT], [B2T])

    if debug == "p0":
        dbg_mod = C.dram("dbg_mod", [128, 48], F32, "ExternalOutput")
        dbg_g1 = C.dram("dbg_g1", [128, 1024], F32, "ExternalOutput")
        dma("sp", dbg_mod[:], modT[:], [modT], [dbg_mod], "dbg1")
        dma("sp", dbg_g1[:], G1bc[:], [G1bc], [dbg_g1], "dbg2")
        P.final_wait("sp", ["dbg_mod", "dbg_g1"])
        P.emit()
        return nc


    C.release(m_phase)
    w_in_sb = C.sb([128, 8, 3592], BF16, "w_in")
    w_in_v = w_in_d[:].rearrange("(kc p) n -> p kc n", p=128)
    for kc in range(8):
        dma("pool", w_in_sb[:, kc, :], w_in_v[:, kc, :], [], [w_in_sb], "w_in")
    dma("sp", convw[:], convw_d[:], [], [convw], "c_convw")
    alog = C.sb([128, 4], F32, "alog")
    dma("sp", alog[:], alog_d[:], [], [alog], "c_alog")
    dma("sp", dtb[:], dtb_d[:], [], [dtb], "c_dtb")
    act(negA[:], alog[:], AF.Exp, [alog], [negA])
    ts("dve", negA[:], negA[:], -1.0, None, ALU.mult, None, [negA], [negA])

    def norm_to_hT(xt, hT, col0, AT, BT, xn_rot, st_rot, pT):
        xn = xn_rot.next()
        st = st_rot.next()
        act(xn[:], xt[:], AF.Square, [xt], [xn, st], accum=st[:, 0:1])
        act(st[:, 1:2], st[:, 0:1], AF.Ln, [st], [st], bias=EPS, scale=1.0 / 1024)
        act(st[:, 2:3], st[:, 1:2], AF.Exp, [st], [st], scale=-0.5)
        act(xn[:], xt[:], AF.Copy, [xt, st], [xn], scale=st[:, 2:3])
        for kc in range(8):
            tr(pT[:, kc * 128:(kc + 1) * 128], xn[:, kc * 128:(kc + 1) * 128], ident_b[:], [xn, ident_b], [pT])
        tmp = st_rot_big.next()
        tt("dve", tmp[:].rearrange("p (k t) -> p k t", k=8), pT[:].rearrange("p (k t) -> p k t", k=8),
           AT[:].to_broadcast([128, 8, 128]) if False else AT[:, :, None].to_broadcast([128, 8, 128]), ALU.mult, [pT, AT], [tmp])
        tt("dve", hT[:, :, col0:col0 + 128], tmp[:].rearrange("p (k t) -> p k t", k=8),
           BT[:, :, None].to_broadcast([128, 8, 128]), ALU.add, [tmp, BT], [hT])

    xt_rot = Rot([C.sb([128, 1024], F32, f"xt{i}") for i in range(2)])
    xn_rot = Rot([C.sb([128, 1024], BF16, f"xn{i}") for i in range(2)])
    st_rot = Rot([C.sb([128, 4], F32, f"st{i}") for i in range(2)])
    st_rot_big = Rot([C.sb([128, 1024], F32, f"hTtmp{i}") for i in range(2)])
    hT_rot = Rot([C.sb([128, 8, 512], BF16, f"hT{i}") for i in range(2)])
    raw = [C.sb([128, 515], F32, f"raw{i}") for i in range(12)]
    for r_ in raw:
        mset("pool", r_[:, 0:3], 0.0, [r_])
    cacc_rot = Rot([C.sb([128, 512], F32, f"cacc{i}") for i in range(3)])
    sq_rot = Rot([C.sb([128, 512], BF16, f"sq{i}") for i in range(2)])
    ob_rot = Rot([C.sb([128, 512], BF16, f"ob{i}") for i in range(3)])
    of_rot = Rot([C.sb([128, 512], F32, f"of{i}") for i in range(3)])
    va_rot = Rot([C.sb([128, 8, 65], BF16, f"va{i}") for i in range(2)])
    for vb_ in va_rot.bufs:
        mset("pool", vb_[:], 1.0, [vb_])
    bgt_rot = Rot([C.sb([128, 16], F32, f"bgt{i}") for i in range(2)])
    ps_rot = Rot(psA)
    psT_rot = Rot(psT)
    x_v = x_ext[:].rearrange("(t p) d -> t p d", p=128)
    C_AQ, C_AK, C_AV, C_DQ, C_DK, C_DV, C_DZ, C_DB, C_DA = 0, 512, 1024, 1536, 2048, 2560, 3072, 3584, 3588

    n_groups = 16 if debug is None else int(os.environ.get("K_NGROUPS", "16"))
    hT_of = {}

    def p1_stage_a(g):
        is_pre = g < 8
        hT = hT_rot.next()
        for ti in range(4):
            t = g * 4 + ti
            xt = xt_rot.next()
            dma("sp", xt[:], x_v[t], [], [xt], f"xt{t % 2}")
            norm_to_hT(xt, hT, ti * 128, A1T, B1pT if is_pre else B1T, xn_rot, st_rot, psT_rot.next())
        hT_of[g] = hT

    p1_stage_a(0)
    for g in range(n_groups):
        is_pre = g < 8
        if g + 1 < n_groups:
            p1_stage_a(g + 1)
        hT = hT_of.pop(g)
        tok0 = g * 512
        mtok0 = tok0 - NPRE
        ktok0 = tok0 - 2048

        def proj_fm(col0):
            pp = ps_rot.next()
            for kc in range(8):
                mm(pp[:], w_in_sb[:, kc, col0:col0 + 128], hT[:, kc, :], kc == 0, kc == 7, [w_in_sb, hT], [pp])
            return pp

        if not is_pre:
            for c4 in range(4):
                pp = proj_fm(C_AQ + c4 * 128)
                ob = ob_rot.next()
                act(ob[:], pp[:], AF.Copy, [pp], [ob], scale=0.125)
                dma("pool", aqT[c4 * 128:(c4 + 1) * 128, mtok0:mtok0 + 512], ob[:], [ob], [("aqT", g)], f"st_{ob.base}")
        if g >= 4:
            for c4 in range(4):
                pp = proj_fm(C_AK + c4 * 128)
                ob = ob_rot.next()
                cp("act", ob[:], pp[:], [pp], [ob])
                dma("pool", akT[c4 * 128:(c4 + 1) * 128, ktok0:ktok0 + 512], ob[:], [ob], [("akT", g)], f"st_{ob.base}")
        for cc in range(12):
            if is_pre and cc < 4 and g != 7:
                continue
            pp = proj_fm(C_DQ + cc * 128)
            rw = raw[cc]
            cp("act", rw[:, 3:515], pp[:], [pp], [rw])
            if is_pre and cc < 4:
                cp("pool", rw[:, 0:3], rw[:, 512:515], [rw], [rw])
                continue
            ca = cacc_rot.next()
            ts("dve", ca[:], rw[:, 3:515], convw[:, cc * 4 + 3:cc * 4 + 4], None, ALU.mult, None, [rw, convw], [ca])
            for j in (2, 1, 0):
                stt("dve", ca[:], rw[:, j:j + 512], convw[:, cc * 4 + j:cc * 4 + j + 1], ca[:], ALU.mult, ALU.add,
                    [rw, convw, ca], [ca])
            cp("pool", rw[:, 0:3], rw[:, 512:515], [rw], [rw])
            of = of_rot.next()
            act(of[:], ca[:], AF.Silu, [ca], [of])
            if cc < 8:
                sq = sq_rot.next()
                act(sq[:], of[:], AF.Square, [of], [sq])
                pn = ps_rot.next()
                mm(pn[:], ones_b[:], sq[:], True, True, [ones_b, sq], [pn])
                rn = cacc_rot.next()
                act(rn[:], pn[:], AF.Ln, [pn], [rn], bias=EPS)
                act(rn[:], rn[:], AF.Exp, [rn], [rn], scale=-0.5)
                sc = (128.0 ** -0.5) if cc < 4 else 1.0
                stt("dve", of[:], of[:], sc, rn[:], ALU.mult, ALU.mult, [of, rn], [of])
            if cc < 4:
                dst = dqT[cc * 128:(cc + 1) * 128, mtok0:mtok0 + 512]
                dk_ = ("dqT", g)
            elif cc < 8:
                dst = dkT[(cc - 4) * 128:(cc - 3) * 128, tok0:tok0 + 512]
                dk_ = ("dkT", g)
            else:
                dst = dvT[(cc - 8) * 128:(cc - 7) * 128, tok0:tok0 + 512]
                dk_ = ("dvT", g)
            dma("pool", dst, of[:], [of], [dk_], f"st_{of.base}")
        for ti in range(4):
            t = g * 4 + ti
            lhs = lambda kc: hT[:, kc, ti * 128:(ti + 1) * 128]
            if g >= 4:
                pp = ps_rot.next()
                for kc in range(8):
                    mm(pp[:], lhs(kc), w_in_sb[:, kc, C_AV:C_AV + 512], kc == 0, kc == 7, [w_in_sb, hT], [pp])
                va = va_rot.next()
                cp("act", va[:, :, 0:64], pp[:].rearrange("p (h e) -> p h e", h=8), [pp], [va])
                dma("pool", avA[ktok0 + ti * 128:ktok0 + (ti + 1) * 128, :], va[:].rearrange("p h e -> p (h e)"),
                    [va], [("avA", t)], f"st_{va.base}")
            if not is_pre:
                pp = ps_rot.next()
                for kc in range(8):
                    mm(pp[:], lhs(kc), w_in_sb[:, kc, C_DZ:C_DZ + 512], kc == 0, kc == 7, [w_in_sb, hT], [pp])
                of = of_rot.next()
                act(of[:], pp[:], AF.Silu, [pp], [of])
                dma("pool", dzs[mtok0 + ti * 128:mtok0 + (ti + 1) * 128, :], of[:], [of], [("dzs", t)], f"st_{of.base}")
            pp = ps_rot.next()
            for kc in range(8):
                mm(pp[:, 0:8], lhs(kc), w_in_sb[:, kc, C_DB:C_DB + 8], kc == 0, kc == 7, [w_in_sb, hT], [pp])
            bgt = bgt_rot.next()
            act(bgt[:, 0:4], pp[:, 0:4], AF.Sigmoid, [pp], [bgt])
            tt("dve", bgt[:, 8:12], pp[:, 4:8], dtb[:], ALU.add, [pp, dtb], [bgt])
            act(bgt[:, 12:16], bgt[:, 8:12], AF.Exp, [bgt], [bgt])
            act(bgt[:, 8:12], bgt[:, 12:16], AF.Ln, [bgt], [bgt], bias=1.0)
            tt("dve", bgt[:, 4:8], bgt[:, 8:12], negA[:], ALU.mult, [bgt, negA], [bgt])
            dma("pool", bg_d[t * 128:(t + 1) * 128, :], bgt[:, 0:8], [bgt], [("bg", t)], f"st_{bgt.base}")

    if debug == "p1":
        P.final_wait("pool", [k for k in P.lastw if isinstance(k, tuple)])
        P.emit()
        return nc

    C.release(m_phase)
    mtmp = C.sb([128, 128], F32, "mtmp")
    mset("pool", mtmp[:], 1.0, [mtmp])
    E("pool", lambda e: e.affine_select(mtmp[:], mtmp[:], [[1, 128]], ALU.is_ge, 0.0, base=0, channel_multiplier=-1),
      [mtmp], [mtmp])
    mtmp2 = C.sb([128, 128], F32, "mtmp2")
    mset("pool", mtmp2[:], 1.0, [mtmp2])
    E("pool", lambda e: e.affine_select(mtmp2[:], mtmp2[:], [[-1, 128]], ALU.is_ge, 0.0, base=0, channel_multiplier=1),
      [mtmp2], [mtmp2])
    mask01 = C.sb([128, 256], BF16, "mask01")
    cp("dve", mask01[:, 0:128], mtmp2[:], [mtmp2], [mask01])
    cp("dve", mask01[:, 128:256], mtmp[:], [mtmp], [mask01])
    mset("pool", sel65[:], 1.0, [sel65])
    E("pool", lambda e: e.affine_select(sel65[:], sel65[:], [[0, 64]], ALU.is_equal, 0.0, base=-64, channel_multiplier=1),
      [sel65], [sel65])

    qs = C.sb([128, 4, 2048], BF16, "qs")
    ks = C.sb([128, 4, 4096], BF16, "ks")
    acc = C.sb([128, 8, 2048], F32, "acc")
    vt_rot = Rot([C.sb([128, 8, 65], BF16, f"vt{i}") for i in range(4)])
    pt_rot = Rot([C.sb([128, 256], BF16, f"pt{i}") for i in range(8)])

    class View2:
        def __init__(self, ap, key):
            self.ap = ap
            self.key = key

        def __getitem__(self, idx):
            return self.ap[idx]

    ps2_rot = Rot(list(psA) + [View2(psT[i][:].bitcast(F32), psT[i].key) for i in range(2)])
    rd_rot = Rot([C.sb([64, 512], F32, f"rd{i}") for i in range(2)])
    mo_rot = Rot([C.sb([64, 512], BF16, f"mo{i}") for i in range(2)])
    n_sb = 2 if debug is None else int(os.environ.get("K_NSB", "2"))
    for sb_ in range(n_sb):
        for p4 in range(4):
            dma("sp", qs[:, p4, :], aqT[p4 * 128:(p4 + 1) * 128, sb_ * 2048:(sb_ + 1) * 2048],
                [("aqT", 8 + sb_ * 4 + i) for i in range(4)], [qs], "qs")
            dma("sp", ks[:, p4, :], akT[p4 * 128:(p4 + 1) * 128, sb_ * 2048:sb_ * 2048 + 4096],
                [("akT", 4 + sb_ * 4 + i) for i in range(8)], [ks], "ks")
        for d in (1, 4, 16):
            nblk = 16 // d
            for r in range(d):
                vts = {}
                for n in range(nblk):
                    for kb in (n - 1, n):
                        if kb in vts:
                            continue
                        idx0 = sb_ * 2048 + 2048 + r + d * 128 * kb
                        vt = vt_rot.next()
                        t_lo = idx0 // 128
                        t_hi = (idx0 + d * 127) // 128
                        dma("sp", vt[:].rearrange("p h e -> p (h e)"), avA[idx0:idx0 + 127 * d + 1:d, :],
                            [("avA", 16 + tt_) for tt_ in range(t_lo, t_hi + 1)], [vt], f"ld_{vt.base}")
                        vts[kb] = vt
                    vprev, vcur = vts[n - 1], vts[n]
                    q0 = r + d * 128 * n
                    kp0 = 2048 + r + d * 128 * (n - 1)
                    kc0 = 2048 + r + d * 128 * n
                    pend = []
                    for h in range(8):
                        p4, r0 = h // 2, (h % 2) * 64
                        qv = qs[r0:r0 + 64, p4, q0:q0 + 127 * d + 1:d]
                        kpv = ks[r0:r0 + 64, p4, kp0:kp0 + 127 * d + 1:d]
                        kcv = ks[r0:r0 + 64, p4, kc0:kc0 + 127 * d + 1:d]
                        pss = ps2_rot.next()
                        mm(pss[:, 0:128], kpv, qv, True, True, [ks, qs], [pss])
                        mm(pss[:, 128:256], kcv, qv, True, True, [ks, qs], [pss])
                        pt = pt_rot.next()
                        if sb_ == 0 and n == 0:
                            act(pt[:, 0:128], pss[:, 0:128], AF.Exp, [pss, flags], [pt], bias=flags[:, 1:2])
                            act(pt[:, 128:256], pss[:, 128:256], AF.Exp, [pss], [pt])
                        else:
                            act(pt[:], pss[:, 0:256], AF.Exp, [pss], [pt])
                        tt("dve", pt[:], pt[:], mask01[:], ALU.mult, [pt, mask01], [pt])

                        def pv_part(h=h, pt=pt):
                            pso = ps2_rot.next()
                            mm(pso[0:65, 0:128], vprev[:, h, :], pt[:, 0:128], True, False, [vprev, pt], [pso])
                            mm(pso[0:65, 0:128], vcur[:, h, :], pt[:, 128:256], False, True, [vcur, pt], [pso])
                            av_ = acc[0:65, h, q0:q0 + 127 * d + 1:d]
                            if d == 1:
                                cp("dve", av_, pso[0:65, 0:128], [pso], [("acc", h)])
                            else:
                                tt("dve", av_, av_, pso[0:65, 0:128], ALU.add, [pso, ("acc", h)], [("acc", h)])
                        pend.append(pv_part)
                        if len(pend) > 2:
                            pend.pop(0)()
                    for f_ in pend:
                        f_()
        for h in range(8):
            for c4 in range(4):
                psd = ps_rot.next()
                mm(psd[0:64, :], sel65[0:65, :], acc[0:65, h, c4 * 512:(c4 + 1) * 512], True, True,
                   [sel65, ("acc", h)], [psd])
                rd = rd_rot.next()
                E("dve", lambda e, rd=rd, psd=psd: e.reciprocal(rd[:], psd[0:64, :]), keys_of([psd]), keys_of([rd]))
                mo = mo_rot.next()
                tt("dve", mo[:], acc[0:64, h, c4 * 512:(c4 + 1) * 512], rd[:], ALU.mult, [rd, ("acc", h)], [mo])
                dma("pool", mixT[h * 64:(h + 1) * 64, sb_ * 2048 + c4 * 512:sb_ * 2048 + (c4 + 1) * 512], mo[:],
                    [mo], [("mixT_a", sb_)], f"st_{mo.base}")

    if debug == "p2":
        P.final_wait("pool", [k for k in P.lastw if isinstance(k, tuple)])
        P.emit()
        return nc

    C.release(m_phase)

    class View:
        def __init__(self, ap, key):
            self.ap = ap
            self.key = key

        def __getitem__(self, idx):
            return self.ap[idx]

    psq = Rot([View(psA[i][:, q * 128:(q + 1) * 128], psA[i].key) for q in range(4) for i in range(6)])
    pst = Rot([View(psT[i][:, q * 128:(q + 1) * 128], psT[i].key) for q in range(4) for i in range(2)])

    Uinc = C.sb([128, 128], F32, "Uinc")
    mset("pool", Uinc[:], 1.0, [Uinc])
    E("pool", lambda e: e.affine_select(Uinc[:], Uinc[:], [[1, 128]], ALU.is_ge, 0.0, base=0, channel_multiplier=-1),
      [Uinc], [Uinc])
    ones_f = C.sb([128, 128], F32, "onesf")
    mset("pool", ones_f[:], 1.0, [ones_f])
    bigS = C.sb([128, 128], F32, "bigS")
    mset("pool", bigS[:], 0.0, [bigS])
    E("pool", lambda e: e.affine_select(bigS[:], bigS[:], [[-1, 128]], ALU.is_ge, 1e4, base=-1, channel_multiplier=1),
      [bigS], [bigS])
    nbigT = C.sb([128, 128], F32, "nbigT")
    mset("pool", nbigT[:], 0.0, [nbigT])
    E("pool", lambda e: e.affine_select(nbigT[:], nbigT[:], [[1, 128]], ALU.is_ge, -1e4, base=0, channel_multiplier=-1),
      [nbigT], [nbigT])
    lm = C.sb([128, 14, 128], BF16, "lm")
    dma("pool", lm[:].rearrange("p a b -> p (a b)"), lvlmask_d[:], [], [lm], "c_lm")
    dnw = C.sb([128, 128], F32, "dnw")
    dma("sp", dnw[:], dnw_d[:], [], [dnw], "c_dnw")
    Sst = [C.sb([128, 128], F32, f"S{h}") for h in range(4)]
    for S_ in Sst:
        mset("pool", S_[:], 0.0, [S_])

    def rot(shape, dt, name, n):
        return Rot([C.sb(shape, dt, f"{name}{i}") for i in range(n)])

    kb_rot = rot([128, 4, 128], BF16, "kb", 3)
    vb4_rot = rot([128, 4, 128], BF16, "vb4_", 3)
    qb_rot = rot([128, 4, 128], BF16, "qb", 3)
    kf_rot = rot([128, 4, 128], F32, "kf", 3)
    vf_rot = rot([128, 4, 128], F32, "vf", 3)
    qf_rot = rot([128, 4, 128], F32, "qf", 3)
    bgt3_rot = rot([128, 8], F32, "bgt3_", 3)
    dzt_rot = rot([128, 512], F32, "dzt", 3)
    gcs_rot = rot([128, 32], F32, "gcs", 3)
    INFLIGHT = int(os.environ.get("K_INFLIGHT", "3"))
    ND = 4 * INFLIGHT
    f32pools = {n: rot([128, 128], F32, n, ND) for n in
                ["grep", "tS", "tT", "DmS", "DmT", "egc", "qdT", "kd", "intraT", "wT", "uu", "vn", "gz"]}
    b16pools = {n: rot([128, 128], BF16, n, ND) for n in
                ["kbg", "vb", "Ab", "ATb", "Zs", "Zs2", "on", "onT"]}
    gh_rings = [rot([128, 128], BF16, f"GH{u}_", 4) for u in range(ND)]
    ul_rings = [rot([128, 128], BF16, f"UL{u}_", 4) for u in range(ND)]
    st3_rot = rot([128, 4], F32, "st3_", ND)
    dk_v = dkT[:].rearrange("(h p) t -> p h t", p=128)
    dv_v = dvT[:].rearrange("(h p) t -> p h t", p=128)
    dq_v = dqT[:].rearrange("(h p) t -> p h t", p=128)

    n_chunks = 64 if debug is None else int(os.environ.get("K_NCHUNKS", "64"))

    def start_chunk(c):
        main = c >= 32
        g_ = c // 4
        Kf4 = kf_rot.next()
        Vf4 = vf_rot.next()
        dma("sp", Kf4[:], dk_v[:, :, c * 128:(c + 1) * 128], [("dkT", g_)], [Kf4], f"ld_{Kf4.base}")
        dma("sp", Vf4[:], dv_v[:, :, c * 128:(c + 1) * 128], [("dvT", g_)], [Vf4], f"ld_{Vf4.base}")
        bgt = bgt3_rot.next()
        dma("sp", bgt[:], bg_d[c * 128:(c + 1) * 128, :], [("bg", c)], [bgt], f"ld_{bgt.base}")
        if main:
            Qf4 = qf_rot.next()
            dma("sp", Qf4[:], dq_v[:, :, (c - 32) * 128:(c - 31) * 128], [("dqT", g_)], [Qf4], f"ld_{Qf4.base}")
            dzt = dzt_rot.next()
            dma("sp", dzt[:], dzs[(c - 32) * 128:(c - 31) * 128, :], [("dzs", c)], [dzt], f"ld_{dzt.base}")
        Kb4 = kb_rot.next()
        Vb4 = vb4_rot.next()
        cp("pool", Kb4[:], Kf4[:], [Kf4], [Kb4])
        cp("pool", Vb4[:], Vf4[:], [Vf4], [Vb4])
        if main:
            Qb4 = qb_rot.next()
            cp("pool", Qb4[:], Qf4[:], [Qf4], [Qb4])
        psg0 = psq.next()
        mm(psg0[:, 0:4], Uinc[:], bgt[:, 4:8], True, True, [Uinc, bgt], [psg0])
        mm(psg0[:, 4:8], ones_f[:], bgt[:, 4:8], True, True, [ones_f, bgt], [psg0])
        gcs = gcs_rot.next()
        cp("act", gcs[:, 0:8], psg0[:, 0:8], [psg0], [gcs])
        act(gcs[:, 20:24], gcs[:, 0:4], AF.Exp, [gcs], [gcs])
        tt("dve", gcs[:, 8:12], gcs[:, 20:24], bgt[:, 0:4], ALU.mult, [gcs, bgt], [gcs])
        tt("dve", gcs[:, 24:28], gcs[:, 4:8], gcs[:, 0:4], ALU.subtract, [gcs], [gcs])
        act(gcs[:, 12:16], gcs[:, 24:28], AF.Exp, [gcs], [gcs])
        act(gcs[:, 16:20], gcs[:, 4:8], AF.Exp, [gcs], [gcs])

        def head_gen(h):
            uslot = (c % INFLIGHT) * 4 + h
            gh_rot = gh_rings[uslot]
            ul_rot = ul_rings[uslot]
            gh_rot.i = 0
            ul_rot.i = 0
            Kf = Kf4[:, h, :]
            Vf = Vf4[:, h, :]
            gcc = gcs[:, h:h + 1]
            beta_c = bgt[:, h:h + 1]
            fp = {n: p_.next() for n, p_ in f32pools.items()}
            bp = {n: p_.next() for n, p_ in b16pools.items()}
            cp("dve", fp["grep"][:], bgt[:, 4 + h:5 + h].to_broadcast([128, 128]), [bgt], [fp["grep"]])
            psg = psq.next()
            mm(psg[:], fp["grep"][:], Uinc[:], True, True, [fp["grep"], Uinc], [psg])
            yield
            _sub = int(os.environ.get("K_P3SUB", "99")) if debug is not None else 99
            if _sub < 1:
                return
            stt("dve", fp["tS"][:], psg[:], gcc, bigS[:], ALU.subtract, ALU.max, [psg, gcs, bigS], [fp["tS"]])
            if _sub < 2:
                return
            act(fp["DmS"][:], fp["tS"][:], AF.Exp, [fp["tS"]], [fp["DmS"]], scale=-1.0)
            if _sub < 3:
                return
            if main:
                stt("dve", fp["tT"][:], psg[:], gcc, nbigT[:], ALU.subtract, ALU.min, [psg, gcs, nbigT], [fp["tT"]])
                act(fp["DmT"][:], fp["tT"][:], AF.Exp, [fp["tT"]], [fp["DmT"]])
                act(fp["egc"][:], psg[:], AF.Exp, [psg], [fp["egc"]])
                tt("pool", fp["qdT"][:], Qf4[:, h, :], fp["egc"][:], ALU.mult, [Qf4, fp["egc"]], [fp["qdT"]])
            Kb = Kb4[:, h, :]
            psk = psq.next()
            mm(psk[:], Kb, ident_b[:], True, True, [Kb4, ident_b], [psk])
            psv = psq.next()
            mm(psv[:], Vb4[:, h, :], ident_b[:], True, True, [Vb4, ident_b], [psv])
            yield
            ts("dve", bp["kbg"][:], psk[:], gcs[:, 8 + h:9 + h], None, ALU.mult, None, [psk, gcs], [bp["kbg"]])
            ts("dve", fp["kd"][:], psk[:], gcs[:, 12 + h:13 + h], None, ALU.mult, None, [psk, gcs], [fp["kd"]])
            ts("dve", bp["vb"][:], psv[:], beta_c, None, ALU.mult, None, [psv, bgt], [bp["vb"]])
            pskk = psq.next()
            mm(pskk[:], Kb, Kb, True, True, [Kb4], [pskk])
            if main:
                psqk = psq.next()
                mm(psqk[:], Kb, Qb4[:, h, :], True, True, [Kb4, Qb4], [psqk])
            yield
            stt("dve", bp["Ab"][:], pskk[:], beta_c, fp["DmS"][:], ALU.mult, ALU.mult, [pskk, bgt, fp["DmS"]], [bp["Ab"]])
            if main:
                tt("dve", fp["intraT"][:], psqk[:], fp["DmT"][:], ALU.mult, [psqk, fp["DmT"]], [fp["intraT"]])
            pt_ = pst.next()
            tr(pt_[:], bp["Ab"][:], ident_b[:], [bp["Ab"], ident_b], [pt_])
            cp("act", bp["ATb"][:], pt_[:], [pt_], [bp["ATb"]])
            yield
            A_, AT_ = bp["Ab"], bp["ATb"]
            G = gh_rot.next()
            H = gh_rot.next()
            tt("dve", G[:], AT_[:], lm[:, 0, :], ALU.mult, [AT_, lm], [G])
            tt("dve", G[:], G[:], ident_b[:], ALU.add, [G, ident_b], [G])
            tt("dve", H[:], A_[:], lm[:, 1, :], ALU.mult, [A_, lm], [H])
            tt("dve", H[:], H[:], ident_b[:], ALU.add, [H, ident_b], [H])
            Ub, Lb = {}, {}

            def mk_masks(k):
                Lb[k] = ul_rot.next()
                tt("pool", Lb[k][:], A_[:], lm[:, 2 * k + 1, :], ALU.mult, [A_, lm], [Lb[k]])
                if k < 6:
                    Ub[k] = ul_rot.next()
                    tt("pool", Ub[k][:], AT_[:], lm[:, 2 * k, :], ALU.mult, [AT_, lm], [Ub[k]])

            mk_masks(1)
            yield
            for k in range(1, 7):
                last = k == 6
                if not last:
                    mk_masks(k + 1)
                psz = psq.next()
                mm(psz[:], Lb[k][:], G[:], True, True, [Lb[k], G], [psz])
                Zs = b16pools["Zs"].next()
                cp("act", Zs[:], psz[:], [psz], [Zs])
                if not last:
                    psz2 = psq.next()
                    mm(psz2[:], Ub[k][:], H[:], True, True, [Ub[k], H], [psz2])
                    Zs2 = b16pools["Zs2"].next()
                    cp("act", Zs2[:], psz2[:], [psz2], [Zs2])
                yield
                psy = psq.next()
                mm(psy[:], H[:], Zs[:], True, True, [H, Zs], [psy])
                Gn = gh_rot.next()
                tt("dve", Gn[:], G[:], psy[:], ALU.subtract, [G, psy], [Gn])
                if not last:
                    psy2 = psq.next()
                    mm(psy2[:], G[:], Zs2[:], True, True, [G, Zs2], [psy2])
                    Hn = gh_rot.next()
                    tt("dve", Hn[:], H[:], psy2[:], ALU.subtract, [H, psy2], [Hn])
                    H = Hn
                else:
                    gh_rot.next()
                G = Gn
                yield
            psw = psq.next()
            mm(psw[:], bp["kbg"][:], G[:], True, True, [bp["kbg"], G], [psw])
            cp("act", fp["wT"][:], psw[:], [psw], [fp["wT"]])
            psu = psq.next()
            mm(psu[:], G[:], bp["vb"][:], True, True, [G, bp["vb"]], [psu])
            cp("act", fp["uu"][:], psu[:], [psu], [fp["uu"]])
            yield
            S_ = Sst[h]
            ps1 = psq.next()
            mm(ps1[:], fp["wT"][:], S_[:], True, True, [fp["wT"], S_], [ps1])
            tt("dve", fp["vn"][:], fp["uu"][:], ps1[:], ALU.subtract, [fp["uu"], ps1], [fp["vn"]])
            yield
            if main:
                pso = psq.next()
                mm(pso[:], fp["qdT"][:], S_[:], True, False, [fp["qdT"], S_], [pso])
                mm(pso[:], fp["intraT"][:], fp["vn"][:], False, True, [fp["intraT"], fp["vn"]], [pso])
            ps2 = psq.next()
            mm(ps2[:], fp["kd"][:], fp["vn"][:], True, True, [fp["kd"], fp["vn"]], [ps2])
            stt("dve", S_[:], S_[:], gcs[:, 16 + h:17 + h], ps2[:], ALU.mult, ALU.add, [S_, gcs, ps2], [S_])
            yield
            if main:
                st = st3_rot.next()
                act(fp["tS"][:], pso[:], AF.Square, [pso], [fp["tS"], st], accum=st[:, 0:1])
                act(st[:, 1:2], st[:, 0:1], AF.Ln, [st], [st], bias=EPS, scale=1.0 / 128)
                act(st[:, 2:3], st[:, 1:2], AF.Exp, [st], [st], scale=-0.5)
                tt("pool", fp["gz"][:], dnw[:], dzt[:, h * 128:(h + 1) * 128], ALU.mult, [dnw, dzt], [fp["gz"]])
                stt("dve", bp["on"][:], pso[:], st[:, 2:3], fp["gz"][:], ALU.mult, ALU.mult, [pso, st, fp["gz"]], [bp["on"]])
                yield
                pt2 = pst.next()
                tr(pt2[:], bp["on"][:], ident_b[:], [bp["on"], ident_b], [pt2])
                cp("act", bp["onT"][:], pt2[:], [pt2], [bp["onT"]])
                m0 = (c - 32) * 128
                dma("pool", mixT[512 + h * 128:512 + (h + 1) * 128, m0:m0 + 128], bp["onT"][:], [bp["onT"]],
                    [("mixT_d", c)], f"st_{bp['onT'].base}")
                if debug == "p3":
                    dma("pool", dbg_o[m0:m0 + 128, h * 128:(h + 1) * 128], bp["on"][:], [bp["on"]], [("dbg_o", c, h)],
                        f"st2_{bp['on'].base}")

        return [head_gen(h) for h in range(4)]

    STAGGER = int(os.environ.get("K_STAGGER", "7"))
    active = []
    next_c = 0
    while next_c < n_chunks or active:
        if next_c < n_chunks and len(active) < INFLIGHT and (not active or active[-1][1] >= STAGGER):
            active.append([start_chunk(next_c), 0])
            next_c += 1
        for ent in active:
            nxt = []
            for g_i in ent[0]:
                try:
                    next(g_i)
                    nxt.append(g_i)
                except StopIteration:
                    pass
            ent[0] = nxt
            ent[1] += 1
        active = [ent for ent in active if ent[0]]

    if debug == "p3":
        P.final_wait("pool", [k for k in P.lastw if isinstance(k, tuple)])
        P.emit()
        return nc

    C.release(m_phase)
    GT = 16
    n_grp = NMAIN // (GT * 128)
    n_exp = 32 if debug is None else int(os.environ.get("K_NEXP", "32"))
    fnw = C.sb([128, 1024], F32, "fnw")
    dma("sp", fnw[:], fnw_d[:], [], [fnw], "c_fnw")
    brt = C.sb([128, 32], F32, "brt")
    dma("sp", brt[:], b_router_d[:], [], [brt], "c_brt")
    wr_sb = C.sb([128, 8, 32], BF16, "wr")
    dma("pool", wr_sb[:], w_router_d[:].rearrange("(kc p) n -> p kc n", p=128), [], [wr_sb], "c_wr")
    ones_row = C.sb([1, 512], BF16, "onesrow")
    mset("pool", ones_row[:], 1.0, [ones_row])
    h2T = C.sb([128, 8, GT * 128], BF16, "h2T")
    Yacc = C.sb([128, GT, 1024], F32, "Yacc")
    gates = C.sb([128, GT, 32], F32, "gates")
    if debug != "p4a":
        b1T = C.sb([128, 512], F32, "b1T")
        dma("sp", b1T[:], b1T_d[:], [], [b1T], "c_b1T")
        b2_sb = C.sb([32, 1024], F32, "b2sb")
        dma("sp", b2_sb[:], b2_d[:], [], [b2_sb], "c_b2")
    m_p4 = C.mark()
    x_main = x_ext[NPRE:NEXT, :].rearrange("(t p) d -> t p d", p=128)
    x2_v = x2_d[:].rearrange("(t p) d -> t p d", p=128)
    out_v = out_d[:].rearrange("(t p) d -> t p d", p=128)
    mix_v = mixT[:].rearrange("(kc p) t -> p kc t", p=128)

    for grp in range(n_grp):
        C.release(m_p4)
        w_out_sb = C.sb([128, 8, 1024], BF16, "w_out")
        for kc in range(8):
            dma("pool", w_out_sb[:, kc, :], w_out_d[kc * 128:(kc + 1) * 128, :], [], [w_out_sb], "w_out")
        mx_rot = rot([128, 8, 128], BF16, "mx", 2)
        xt_rot = rot([128, 1024], F32, "xt4_", 2)
        x2_rot = rot([128, 1024], F32, "x2t", 2)
        xn_rot = rot([128, 1024], BF16, "xn4_", 2)
        st_rot = rot([128, 4], F32, "st4_", 2)
        st_rot_big = rot([128, 1024], F32, "hTtmp4_", 2)
        lg_rot = rot([128, 32], F32, "lg", 2)
        m8_rot = rot([128, 16], F32, "m8", 2)
        pend_tail = None
        for T in range(GT):
            t = grp * GT + T
            tok0 = t * 128
            mx = mx_rot.next()
            dma("sp", mx[:], mix_v[:, :, tok0:tok0 + 128],
                [("mixT_a", tok0 // 2048)] + [("mixT_d", 32 + t)], [mx], f"ld_{mx.base}")
            xt = xt_rot.next()
            dma("sp", xt[:], x_main[t], [], [xt], f"ld_{xt.base}")
            x2t = x2_rot.next()
            for nh in range(2):
                py = ps_rot.next()
                for kc in range(8):
                    mm(py[:], mx[:, kc, :], w_out_sb[:, kc, nh * 512:(nh + 1) * 512], kc == 0, kc == 7,
                       [mx, w_out_sb], [py])
                tt("dve", x2t[:, nh * 512:(nh + 1) * 512], py[:], G1bc[:, nh * 512:(nh + 1) * 512], ALU.mult,
                   [py, G1bc], [x2t])
            tt("pool", x2t[:], x2t[:], xt[:], ALU.add, [x2t, xt], [x2t])
            dma("pool", x2_v[t], x2t[:], [x2t], [("x2", t)], f"st_{x2t.base}")
            xn = xn_rot.next()
            st = st_rot.next()
            act(xn[:], x2t[:], AF.Square, [x2t], [xn, st], accum=st[:, 0:1])
            act(st[:, 1:2], st[:, 0:1], AF.Ln, [st], [st], bias=EPS, scale=1.0 / 1024)
            act(st[:, 2:3], st[:, 1:2], AF.Exp, [st], [st], scale=-0.5)
            act(xn[:], x2t[:], AF.Copy, [x2t, st], [xn], scale=st[:, 2:3])

            def tile_tail(T=T, xn=xn):
                pT = psT_rot.next()
                for kc in range(8):
                    tr(pT[:, kc * 128:(kc + 1) * 128], xn[:, kc * 128:(kc + 1) * 128], ident_b[:], [xn, ident_b], [pT])
                tmp = st_rot_big.next()
                tt("dve", tmp[:].rearrange("p (k t) -> p k t", k=8), pT[:].rearrange("p (k t) -> p k t", k=8),
                   A2T[:, :, None].to_broadcast([128, 8, 128]), ALU.mult, [pT, A2T], [tmp])
                tt("dve", h2T[:, :, T * 128:(T + 1) * 128], tmp[:].rearrange("p (k t) -> p k t", k=8),
                   B2T[:, :, None].to_broadcast([128, 8, 128]), ALU.add, [tmp, B2T], [h2T])
                pl = ps_rot.next()
                for kc in range(8):
                    mm(pl[:, 0:32], h2T[:, kc, T * 128:(T + 1) * 128], wr_sb[:, kc, :], kc == 0, kc == 7, [h2T, wr_sb], [pl])
                lg = lg_rot.next()
                m8 = m8_rot.next()
                tt("dve", lg[:], pl[:, 0:32], brt[:], ALU.add, [pl, brt], [lg])
                E("dve", lambda e, m8=m8, lg=lg: e.max(m8[:, 0:8], lg[:]), keys_of([lg]), keys_of([m8]))
                ts("dve", m8[:, 8:9], m8[:, 0:1], -1.0, None, ALU.mult, None, [m8], [m8])
                gt_ = gates[:, T, :]
                act(gt_, lg[:], AF.Exp, [lg, m8], [("gates", T)], bias=m8[:, 8:9])
                ts("dve", lg[:], lg[:], m8[:, 3:4], None, ALU.is_ge, None, [lg, m8], [lg])
                tt("dve", gt_, gt_, lg[:], ALU.mult, [lg, ("gates", T)], [("gates", T)])
                E("dve", lambda e, m8=m8, gt_=gt_: e.reduce_sum(m8[:, 9:10], gt_, AX.X), keys_of([("gates", T)]), keys_of([m8]))
                E("dve", lambda e, m8=m8: e.reciprocal(m8[:, 10:11], m8[:, 9:10]), keys_of([m8]), keys_of([m8]))
                ts("dve", gt_, gt_, m8[:, 10:11], None, ALU.mult, None, [m8, ("gates", T)], [("gates", T)])
            if pend_tail is not None:
                pend_tail()
            pend_tail = tile_tail
        pend_tail()
        pend_tail = None
        if debug == "p4a":
            dbg_g = C.dram(f"dbg_gates{grp}", [128, GT * 32], F32, "ExternalOutput")
            dma("sp", dbg_g[:], gates[:].rearrange("p t e -> p (t e)"), [("gates", T_) for T_ in range(GT)], [dbg_g.key], "dbgg")
            continue
        C.release(m_p4)
        w1g_rot = rot([128, 8, 512], BF16, "w1g", 2)
        w1u_rot = rot([128, 8, 512], BF16, "w1u", 2)
        w2h_rot = rot([128, 4, 1024], BF16, "w2h", 2)
        gT_rot = rot([32, 128], F32, "gT", 2)
        for T in range(GT):
            pgt = ps_rot.next()
            mm(pgt[0:32, 0:128], gates[:, T, :], ident_f[:], True, True, [("gates", T), ident_f], [pgt])
            gT = gT_rot.next()
            cp("act", gT[:], pgt[0:32, 0:128], [pgt], [gT])
            for nh in range(2):
                pb = ps_rot.next()
                mm(pb[:], gT[:], b2_sb[:, nh * 512:(nh + 1) * 512], True, True, [gT, b2_sb], [pb])
                cp("act", Yacc[:, T, nh * 512:(nh + 1) * 512], pb[:], [pb], [("Yacc", T, nh)])
        actT_rot = rot([128, 4, 512], BF16, "actT", 2)
        g_rot = rot([128, 512], F32, "gg", 2)
        s_rot = rot([128, 512], F32, "ss", 2)
        u_rot = rot([128, 512], F32, "uu4_", 2)
        pend_y = None
        ps8_rot = Rot(list(psA) + [View(psT[i][:].bitcast(F32), psT[i].key) for i in range(2)])
        for e_ in range(n_exp):
            for hf in range(2):
                w1g = w1g_rot.next()
                w1u = w1u_rot.next()
                w2h = w2h_rot.next()
                for kc in range(8):
                    dma("pool", w1g[:, kc, :], w1_d[e_, kc * 128:(kc + 1) * 128, hf * 512:(hf + 1) * 512], [], [w1g], f"ld_{w1g.base}")
                    dma("pool", w1u[:, kc, :], w1_d[e_, kc * 128:(kc + 1) * 128, 1024 + hf * 512:1024 + (hf + 1) * 512], [], [w1u], f"ld_{w1u.base}")
                for fc in range(4):
                    dma("pool", w2h[:, fc, :], w2_d[e_, hf * 512 + fc * 128:hf * 512 + (fc + 1) * 128, :], [], [w2h], f"ld_{w2h.base}")
                for sub in range(GT // 4):
                    actT = actT_rot.next()
                    cs = slice(sub * 512, (sub + 1) * 512)
                    for fc in range(4):
                        pg = ps8_rot.next()
                        for kc in range(8):
                            mm(pg[:], w1g[:, kc, fc * 128:(fc + 1) * 128], h2T[:, kc, cs], kc == 0, kc == 7, [w1g, h2T], [pg])
                        pu = ps8_rot.next()
                        for kc in range(8):
                            mm(pu[:], w1u[:, kc, fc * 128:(fc + 1) * 128], h2T[:, kc, cs], kc == 0, kc == 7, [w1u, h2T], [pu])
                        gg = g_rot.next()
                        ss_ = s_rot.next()
                        uu_ = u_rot.next()
                        bg_col = b1T[:, e_ * 16 + hf * 4 + fc:e_ * 16 + hf * 4 + fc + 1]
                        bu_col = b1T[:, e_ * 16 + 8 + hf * 4 + fc:e_ * 16 + 8 + hf * 4 + fc + 1]
                        ts("dve", gg[:], pg[:], bg_col, 7.0, ALU.add, ALU.min, [pg, b1T], [gg])
                        act(ss_[:], gg[:], AF.Sigmoid, [gg], [ss_], scale=1.702)
                        ts("dve", uu_[:], pu[:], bu_col, 7.0, ALU.add, ALU.min, [pu, b1T], [uu_])
                        ts("dve", uu_[:], uu_[:], -7.0, 1.0, ALU.max, ALU.add, [uu_], [uu_])
                        tt("dve", gg[:], gg[:], ss_[:], ALU.mult, [gg, ss_], [gg])
                        tt("dve", actT[:, fc, :], uu_[:], gg[:], ALU.mult, [uu_, gg], [actT])
                        if fc == 0 and pend_y is not None:
                            pend_y()
                            pend_y = None

                    def y_part(e_=e_, hf=hf, sub=sub, actT=actT, w2h=w2h):
                        for ti in range(4):
                            T = sub * 4 + ti
                            for nh in range(2):
                                py = ps8_rot.next()
                                for fc in range(4):
                                    mm(py[:], actT[:, fc, ti * 128:(ti + 1) * 128], w2h[:, fc, nh * 512:(nh + 1) * 512],
                                       fc == 0, fc == 3, [actT, w2h], [py])
                                ya = Yacc[:, T, nh * 512:(nh + 1) * 512]
                                gcol = gates[:, T, e_:e_ + 1]
                                stt("dve", ya, py[:], gcol, ya, ALU.mult, ALU.add, [py, ("gates", T), ("Yacc", T, nh)], [("Yacc", T, nh)])
                    pend_y = y_part
        if pend_y is not None:
            pend_y()
            pend_y = None
        C.release(m_p4)
        x2r_rot = rot([128, 1024], F32, "x2r", 2)
        x3_rot = rot([128, 1024], F32, "x3_", 2)
        ot_rot = rot([128, 1024], F32, "ot", 2)
        jk_rot = rot([128, 1024], BF16, "jk", 2)
        st5_rot = rot([128, 4], F32, "st5_", 2)
        for T in range(GT):
            t = grp * GT + T
            x2r = x2r_rot.next()
            dma("sp", x2r[:], x2_v[t], [("x2", t)], [x2r], f"ld_{x2r.base}")
            x3 = x3_rot.next()
            tt("pool", x3[:], Yacc[:, T, :], G2bc[:], ALU.mult, [("Yacc", T, 0), ("Yacc", T, 1), G2bc], [x3])
            tt("dve", x3[:], x3[:], x2r[:], ALU.add, [x3, x2r], [x3])
            st = st5_rot.next()
            jk = jk_rot.next()
            act(jk[:], x3[:], AF.Square, [x3], [jk, st], accum=st[:, 0:1])
            act(st[:, 1:2], st[:, 0:1], AF.Ln, [st], [st], bias=EPS, scale=1.0 / 1024)
            act(st[:, 2:3], st[:, 1:2], AF.Exp, [st], [st], scale=-0.5)
            ot = ot_rot.next()
            stt("dve", ot[:], x3[:], st[:, 2:3], fnw[:], ALU.mult, ALU.mult, [x3, st, fnw], [ot])
            dma("pool", out_v[t], ot[:], [ot], ["out"], f"st_{ot.base}")

    if debug == "p4a":
        P.final_wait("sp", [k for k in P.lastw if isinstance(k, tuple)] + [f"dbg_gates{g_}" for g_ in range(n_grp)])
        P.emit()
        return nc
    P.final_wait("sp", ["out"])
    P.emit()
    return nc


def level_masks():
    out = np.zeros((128, 14, 128), np.float32)
    j = np.arange(128)[:, None]
    c = np.arange(128)[None, :]
    for k in range(7):
        b = 1 << k
        mU = ((j // (2 * b)) == (c // (2 * b))) & ((j % (2 * b)) < b) & ((c % (2 * b)) >= b)
        sgn = -1.0 if k == 0 else 1.0
        out[:, 2 * k, :] = sgn * mU
        out[:, 2 * k + 1, :] = sgn * mU.T
    return np.ascontiguousarray(out.reshape(128, 14 * 128))


def make_in_maps(inputs):
    x = np.asarray(inputs["x"], np.float32)
    c = np.asarray(inputs["c"], np.float32)
    f32 = lambda a: np.ascontiguousarray(np.asarray(a, np.float32))

    def colT(v):
        return f32(np.asarray(v).reshape(-1, 128).T)

    def bc(v, n=128):
        v = np.asarray(v, np.float32).reshape(1, -1)
        return f32(np.broadcast_to(v, (n, v.shape[1])))

    shared = {
        "w_ada": f32(inputs["w_ada"][0]),
        "b_adaT": colT(inputs["b_ada"][0]),
        "b_ada_row": f32(inputs["b_ada"][0].reshape(1, -1)),
        "n1T": colT(inputs["norm1_w"][0]),
        "n2T": colT(inputs["norm2_w"][0]),
        "w_in": f32(inputs["w_in"][0]),
        "convw": f32(np.asarray(inputs["conv_w"][0]).reshape(4, 12, 128).transpose(2, 1, 0).reshape(128, 48)),
        "alog_bc": bc(inputs["A_log"][0]),
        "dtb_bc": bc(inputs["dt_bias"][0]),
        "dnw_bc": bc(inputs["dn_norm_w"][0]),
        "w_out": f32(inputs["w_out"][0]),
        "w_router": f32(inputs["w_router"][0]),
        "b_router_bc": bc(inputs["b_router"][0]),
        "w1": f32(inputs["w1"][0]),
        "b1T": f32(np.asarray(inputs["b1"][0]).reshape(32, 16, 128).transpose(2, 0, 1).reshape(128, 512)),
        "w2": f32(inputs["w2"][0]),
        "b2": f32(inputs["b2"][0]),
        "fnw_bc": bc(inputs["final_norm_w"]),
        "lvlmask": level_masks(),
    }
    maps = []
    for core in range(8):
        b, half = core // 2, core % 2
        xe = np.zeros((NEXT, 1024), np.float32)
        if half == 0:
            xe[NPRE:] = x[b, :NMAIN]
        else:
            xe[:] = x[b]
        fl = np.zeros((128, 2), np.float32)
        fl[:, 0] = 1.0 if half == 1 else 0.0
        fl[:, 1] = 0.0 if half == 1 else NEG
        m = dict(shared)
        m["x_ext"] = xe
        m["cT"] = colT(c[b])
        m["flags"] = fl
        maps.append(m)
    return maps


def kernel(**inputs):
    nc = build()
    maps = make_in_maps(inputs)
    res = run_bass_kernel_spmd(nc, maps, core_ids=list(range(8)))
    out = np.zeros((4, 8192, 1024), np.float32)
    for core in range(8):
        b, half = core // 2, core % 2
        out[b, half * NMAIN:(half + 1) * NMAIN] = res.results[core]["out"]
    return out
```
